# Optimizing a Trainium2 kernel written in Bass

```python
import math
import jax, jax.numpy as jnp
from jax import lax
import numpy as np

D_MODEL = 1024
BATCH = 8
SEQ = 4096
DEPTH = 2

MEM_LEN = 256
N_A_LAYERS = (DEPTH + 1) // 2
N_B_LAYERS = DEPTH // 2

D_MIX_SEQ = 3 * D_MODEL // 4
D_MIX_MEM = D_MODEL // 4
MEM_HEADS = 4
MEM_HEAD_DIM = D_MIX_MEM // MEM_HEADS

S5_GROUP = 16
S5_GROUPS = D_MIX_SEQ // S5_GROUP
S5_STATE = 64
S5_DT_MIN = 1e-3
S5_DT_MAX = 1e-1

RWKV_HEAD = 64
RWKV_HEADS = D_MIX_SEQ // RWKV_HEAD
DECAY_LORA = 64
AAA_LORA = 64
GATE_LORA = 128
RWKV_SHIFT_COLS = 3 * D_MIX_SEQ + DECAY_LORA + AAA_LORA + GATE_LORA
RWKV_IN_COLS = RWKV_SHIFT_COLS + D_MIX_MEM
LN_X_EPS = 64e-5

D_FF_DENSE = ((8 * D_MODEL // 3 + 127) // 128) * 128
N_EXPERTS = 8
TOP_K = 2
D_FF_EXPERT = 7 * D_MODEL // 2
MOE_BLOCK = 256
RMS_EPS = 1e-6

kernel_name = "hybrid_s5_rwkv7_memxattn_moe"


def rmsnorm(x, g):
    xf = x.astype(jnp.float32)
    y = xf * lax.rsqrt(jnp.mean(xf * xf, axis=-1, keepdims=True) + RMS_EPS)
    return (y * g.astype(jnp.float32)).astype(x.dtype)


def s5_mixer(u, lam_re, lam_im, log_dt, b_re, b_im, c_re, c_im, d_skip, w_glu, b_glu):
    f32 = jnp.float32
    bsz, seq, _ = u.shape
    ug = u.reshape(bsz, seq, S5_GROUPS, S5_GROUP).astype(f32)
    dt = jnp.exp(log_dt.astype(f32))[:, None]
    lr = lam_re.astype(f32)
    li = lam_im.astype(f32)
    mag = jnp.exp(lr * dt)
    ang = li * dt
    ab_re = mag * jnp.cos(ang)
    ab_im = mag * jnp.sin(ang)
    n_re = ab_re - 1.0
    n_im = ab_im
    den = lr * lr + li * li
    z_re = (n_re * lr + n_im * li) / den
    z_im = (n_im * lr - n_re * li) / den
    br = b_re.astype(f32)
    bi = b_im.astype(f32)
    bb_re = z_re[..., None] * br - z_im[..., None] * bi
    bb_im = z_re[..., None] * bi + z_im[..., None] * br
    bu_re = jnp.einsum('bsgj,gpj->bsgp', ug, bb_re)
    bu_im = jnp.einsum('bsgj,gpj->bsgp', ug, bb_im)
    a_re = jnp.broadcast_to(ab_re, (1, seq) + ab_re.shape)
    a_im = jnp.broadcast_to(ab_im, (1, seq) + ab_im.shape)

    def combine(left, right):
        ar_l, ai_l, xr_l, xi_l = left
        ar_r, ai_r, xr_r, xi_r = right
        ar = ar_r * ar_l - ai_r * ai_l
        ai = ar_r * ai_l + ai_r * ar_l
        xr = ar_r * xr_l - ai_r * xi_l + xr_r
        xi = ar_r * xi_l + ai_r * xr_l + xi_r
        return ar, ai, xr, xi

    _, _, xr, xi = lax.associative_scan(combine, (a_re, a_im, bu_re, bu_im), axis=1)
    y = (jnp.einsum('bsgp,gjp->bsgj', xr, c_re.astype(f32))
         - jnp.einsum('bsgp,gjp->bsgj', xi, c_im.astype(f32))
         + d_skip.astype(f32) * ug)
    y = jax.nn.gelu(y)
    y = y * jax.nn.sigmoid(jnp.einsum('bsgj,gjk->bsgk', y, w_glu.astype(f32)) + b_glu.astype(f32))
    return y.reshape(bsz, seq, D_MIX_SEQ).astype(u.dtype)


def rwkv7_mixer(p, mu, w0, w2, a0, a2, g2, k_k, k_a, r_k, lnx_w, lnx_b):
    f32 = jnp.float32
    bsz, seq, _ = p.shape
    H, N, C = RWKV_HEADS, RWKV_HEAD, D_MIX_SEQ
    p_prev = jnp.pad(p, ((0, 0), (1, 0), (0, 0)))[:, :-1]
    p = p + (p_prev - p) * mu
    r, k, v, wd, ad, gd = jnp.split(
        p, [C, 2 * C, 3 * C, 3 * C + DECAY_LORA, 3 * C + DECAY_LORA + AAA_LORA], axis=-1)
    w_log = -jax.nn.softplus(-(w0 + jnp.tanh(wd) @ w2)) - 0.5
    decay = jnp.exp(-jnp.exp(w_log.astype(f32)))
    a = jax.nn.sigmoid(a0 + ad @ a2)
    g = jax.nn.sigmoid(gd) @ g2
    kk = (k * k_k).astype(f32).reshape(bsz, seq, H, N)
    kk = kk / jnp.maximum(jnp.sqrt(jnp.sum(kk * kk, axis=-1, keepdims=True)), 1e-12)
    k = k * (1 + (a - 1) * k_a)

    def heads(t):
        return t.astype(f32).reshape(bsz, seq, H, N)

    r_h, k_h, v_h, a_h, w_h = heads(r), heads(k), heads(v), heads(a), heads(decay)
    tm = lambda t: jnp.transpose(t, (1, 0, 2, 3))
    xs = (tm(r_h), tm(w_h), tm(k_h), tm(v_h), tm(-kk), tm(kk * a_h))

    def step(state, inp):
        r_t, w_t, k_t, v_t, a_t, b_t = inp
        sa = jnp.einsum('bhij,bhj->bhi', state, a_t)
        state = (state * w_t[:, :, None, :]
                 + sa[..., None] * b_t[:, :, None, :]
                 + v_t[..., None] * k_t[:, :, None, :])
        y_t = jnp.einsum('bhij,bhj->bhi', state, r_t)
        return state, y_t

    s0 = jnp.zeros((bsz, H, N, N), f32)
    _, y = lax.scan(step, s0, xs)
    y = tm(y)
    mean = jnp.mean(y, axis=-1, keepdims=True)
    var = jnp.mean(jnp.square(y - mean), axis=-1, keepdims=True)
    y = ((y - mean) * lax.rsqrt(var + LN_X_EPS)).reshape(bsz, seq, C)
    y = y * lnx_w.astype(f32) + lnx_b.astype(f32)
    bonus = jnp.sum(r_h * k_h * r_k.astype(f32), axis=-1, keepdims=True) * v_h
    y = (y + bonus.reshape(bsz, seq, C)) * g.astype(f32)
    return y.astype(p.dtype)


def memory_cross_attn(q, memn, w_kv):
    bsz, seq, _ = q.shape
    kv = memn @ w_kv
    k, v = jnp.split(kv, 2, axis=-1)
    qh = q.reshape(bsz, seq, MEM_HEADS, MEM_HEAD_DIM)
    kh = k.reshape(bsz, -1, MEM_HEADS, MEM_HEAD_DIM)
    vh = v.reshape(bsz, -1, MEM_HEADS, MEM_HEAD_DIM)
    s = jnp.einsum('bshd,bmhd->bhsm', qh, kh).astype(jnp.float32) * (MEM_HEAD_DIM ** -0.5)
    pr = jax.nn.softmax(s, axis=-1).astype(vh.dtype)
    o = jnp.einsum('bhsm,bmhd->bshd', pr, vh)
    return o.reshape(bsz, seq, D_MIX_MEM)


def swiglu(h, w_gate, w_up, w_down):
    return (jax.nn.silu(h @ w_gate) * (h @ w_up)) @ w_down


def moe_swiglu(h, w_router, b_router, w_gate, w_up, w_down):
    f32 = jnp.float32
    bsz, seq, d = h.shape
    hf = h.reshape(-1, d)
    n_tok = hf.shape[0]
    n_asg = n_tok * TOP_K
    logits = (hf @ w_router).astype(f32) + b_router.astype(f32)
    top_logit, top_idx = lax.top_k(logits, TOP_K)
    top_w = jax.nn.softmax(top_logit, axis=-1)
    e_flat = top_idx.reshape(-1).astype(jnp.int32)
    tok_flat = jnp.repeat(jnp.arange(n_tok, dtype=jnp.int32), TOP_K)
    w_flat = top_w.reshape(-1)
    order = jnp.argsort(e_flat)
    e_sorted = e_flat[order]
    tok_sorted = tok_flat[order]
    w_sorted = w_flat[order]
    counts = jnp.bincount(e_flat, length=N_EXPERTS).astype(jnp.int32)
    starts = jnp.cumsum(counts) - counts
    padded = ((counts + MOE_BLOCK - 1) // MOE_BLOCK) * MOE_BLOCK
    pad_ends = jnp.cumsum(padded)
    pad_starts = pad_ends - padded
    rank = jnp.arange(n_asg, dtype=jnp.int32) - starts[e_sorted]
    dest = pad_starts[e_sorted] + rank
    n_blocks = -(-n_asg // MOE_BLOCK) + N_EXPERTS
    n_rows = n_blocks * MOE_BLOCK
    row_tok = jnp.zeros((n_rows,), jnp.int32).at[dest].set(tok_sorted)
    row_w = jnp.zeros((n_rows,), f32).at[dest].set(w_sorted)
    block_start = jnp.arange(n_blocks, dtype=jnp.int32) * MOE_BLOCK
    block_expert = jnp.minimum(jnp.searchsorted(pad_ends, block_start, side='right'),
                               N_EXPERTS - 1).astype(jnp.int32)
    xb = hf[row_tok].reshape(n_blocks, MOE_BLOCK, d)

    def expert_block(args):
        xblk, e = args
        return swiglu(xblk, w_gate[e], w_up[e], w_down[e])

    yb = lax.map(expert_block, (xb, block_expert)).reshape(n_rows, d)
    yb = yb * row_w[:, None].astype(yb.dtype)
    out = jnp.zeros_like(hf).at[row_tok].add(yb)
    return out.reshape(bsz, seq, d)


def setup_inputs(seed: int = 0) -> dict:
    key = jax.random.key(seed)
    ks = iter(list(jax.random.split(key, 48)))
    f32 = jnp.float32

    def nrm(shape, scale):
        return jax.random.normal(next(ks), shape, f32) * scale

    def gain(shape):
        return 1.0 + nrm(shape, 0.02)

    def unif(shape, lo, hi):
        return jax.random.uniform(next(ks), shape, f32, lo, hi)

    D, G, P, J, C = D_MODEL, S5_GROUPS, S5_STATE, S5_GROUP, D_MIX_SEQ
    na, nb = N_A_LAYERS, N_B_LAYERS
    E, FE, FD = N_EXPERTS, D_FF_EXPERT, D_FF_DENSE
    inp = {}
    inp["x"] = nrm((BATCH, SEQ, D), 1.0)
    inp["mem"] = nrm((BATCH, MEM_LEN, D), 1.0)
    inp["norm_mix"] = gain((DEPTH, D))
    inp["norm_mem"] = gain((DEPTH, D))
    inp["w_kv_mem"] = nrm((DEPTH, D, 2 * D_MIX_MEM), D ** -0.5)
    inp["w_out"] = nrm((DEPTH, D_MIX_SEQ + D_MIX_MEM, D), (D_MIX_SEQ + D_MIX_MEM) ** -0.5)
    inp["norm_ffn"] = gain((DEPTH, D))
    inp["norm_final"] = gain((D,))
    inp["w_in_a"] = nrm((na, D, D_MIX_SEQ + D_MIX_MEM), D ** -0.5)
    inp["s5_lam_re"] = -0.5 + nrm((na, G, P), 0.01)
    inp["s5_lam_im"] = math.pi * jnp.arange(P, dtype=f32) + nrm((na, G, P), 0.01)
    inp["s5_log_dt"] = unif((na, G), math.log(S5_DT_MIN), math.log(S5_DT_MAX))
    inp["s5_b_re"] = nrm((na, G, P, J), (2 * J) ** -0.5)
    inp["s5_b_im"] = nrm((na, G, P, J), (2 * J) ** -0.5)
    inp["s5_c_re"] = nrm((na, G, J, P), P ** -0.5)
    inp["s5_c_im"] = nrm((na, G, J, P), P ** -0.5)
    inp["s5_d"] = nrm((na, G, J), 1.0)
    inp["s5_w_glu"] = nrm((na, G, J, J), J ** -0.5)
    inp["s5_b_glu"] = nrm((na, G, J), 0.01)
    inp["ffn_w_gate"] = nrm((na, D, FD), D ** -0.5)
    inp["ffn_w_up"] = nrm((na, D, FD), D ** -0.5)
    inp["ffn_w_down"] = nrm((na, FD, D), FD ** -0.5)
    inp["w_in_b"] = nrm((nb, D, RWKV_IN_COLS), D ** -0.5)
    inp["rw_mu"] = unif((nb, RWKV_SHIFT_COLS), 0.0, 1.0)
    inp["rw_w0"] = unif((nb, C), -6.0, 1.0)
    inp["rw_w2"] = nrm((nb, DECAY_LORA, C), 0.1 * DECAY_LORA ** -0.5)
    inp["rw_a0"] = nrm((nb, C), 0.1)
    inp["rw_a2"] = nrm((nb, AAA_LORA, C), AAA_LORA ** -0.5)
    inp["rw_g2"] = nrm((nb, GATE_LORA, C), GATE_LORA ** -0.5)
    inp["rw_k_k"] = 0.85 + nrm((nb, C), 0.02)
    inp["rw_k_a"] = 1.0 + nrm((nb, C), 0.02)
    inp["rw_r_k"] = nrm((nb, RWKV_HEADS, RWKV_HEAD), 0.1)
    inp["rw_lnx_w"] = gain((nb, C))
    inp["rw_lnx_b"] = nrm((nb, C), 0.01)
    inp["moe_w_router"] = nrm((nb, D, E), D ** -0.5)
    inp["moe_b_router"] = nrm((nb, E), 0.01)
    inp["moe_w_gate"] = nrm((nb, E, D, FE), D ** -0.5)
    inp["moe_w_up"] = nrm((nb, E, D, FE), D ** -0.5)
    inp["moe_w_down"] = nrm((nb, E, FE, D), FE ** -0.5)
    return inp


def reference(x, mem, norm_mix, norm_mem, w_kv_mem, w_out, norm_ffn, norm_final,
              w_in_a, s5_lam_re, s5_lam_im, s5_log_dt, s5_b_re, s5_b_im, s5_c_re, s5_c_im,
              s5_d, s5_w_glu, s5_b_glu, ffn_w_gate, ffn_w_up, ffn_w_down,
              w_in_b, rw_mu, rw_w0, rw_w2, rw_a0, rw_a2, rw_g2, rw_k_k, rw_k_a, rw_r_k,
              rw_lnx_w, rw_lnx_b, moe_w_router, moe_b_router, moe_w_gate, moe_w_up, moe_w_down):
    for i in range(DEPTH):
        j = i // 2
        h = rmsnorm(x, norm_mix[i])
        memn = rmsnorm(mem, norm_mem[i])
        if i % 2 == 0:
            p = h @ w_in_a[j]
            mix = s5_mixer(p[..., :D_MIX_SEQ], s5_lam_re[j], s5_lam_im[j], s5_log_dt[j],
                           s5_b_re[j], s5_b_im[j], s5_c_re[j], s5_c_im[j], s5_d[j],
                           s5_w_glu[j], s5_b_glu[j])
            q = p[..., D_MIX_SEQ:]
        else:
            p = h @ w_in_b[j]
            mix = rwkv7_mixer(p[..., :RWKV_SHIFT_COLS], rw_mu[j], rw_w0[j], rw_w2[j],
                              rw_a0[j], rw_a2[j], rw_g2[j], rw_k_k[j], rw_k_a[j],
                              rw_r_k[j], rw_lnx_w[j], rw_lnx_b[j])
            q = p[..., RWKV_SHIFT_COLS:]
        ca = memory_cross_attn(q, memn, w_kv_mem[i])
        x = x + jnp.concatenate([mix, ca], axis=-1) @ w_out[i]
        h2 = rmsnorm(x, norm_ffn[i])
        if i % 2 == 0:
            x = x + swiglu(h2, ffn_w_gate[j], ffn_w_up[j], ffn_w_down[j])
        else:
            x = x + moe_swiglu(h2, moe_w_router[j], moe_b_router[j],
                               moe_w_gate[j], moe_w_up[j], moe_w_down[j])
    return rmsnorm(x, norm_final)
```

```python
import contextlib
import math
import numpy as np
import concourse.bass as bass
import concourse.mybir as mybir
from concourse.bass_utils import run_bass_kernel_spmd

F32 = mybir.dt.float32
BF16 = mybir.dt.bfloat16
I32 = mybir.dt.int32
AF = mybir.ActivationFunctionType
ALU = mybir.AluOpType
AX = mybir.AxisListType

NCORES = 8
D = 1024
S = 4096
MEM = 256
NT = S // 128
NTC = S // 512
C_MIX = 768
FD = 2816
FE = 3584
NE = 8


class Trk:
    __slots__ = ("w", "r")

    def __init__(self):
        self.w = {}
        self.r = {}


class T:
    def __init__(self, t, name):
        self.t = t
        self.name = name
        self.trk = Trk()
        self.dsem = {}
        self.is_psum = False

    def __getitem__(self, idx):
        return self.t[idx]


def _isap(x):
    return hasattr(x, "tensor") and hasattr(x, "ap")


class Prog:
    def __init__(self, same_engine_wait=True):
        self.nc = bass.Bass("TRN2", target_bir_lowering=False)
        self.es = contextlib.ExitStack()
        nc = self.nc
        self.eng = {"pe": nc.tensor, "act": nc.scalar, "dve": nc.vector, "pool": nc.gpsimd, "sp": nc.sync}
        self.esem, self.ecnt, self.waited = {}, {}, {}
        for e in self.eng:
            self.esem[e] = self.es.enter_context(nc.semaphore("es_" + e))
            self.ecnt[e] = 0
            self.waited[e] = {}
        self.sew = same_engine_wait
        self.ninst = 0
        self.byname = {}
        self.uid = 0
        self.prefix = ""
        self.sem_es = self.es
        self.dsem_pool = {}
        self.dsem_latest = {}
        self.phase_tiles = []
        self.cache = {}

    def _reg(self, t, name):
        w = T(t, name)
        self.byname[name] = w
        self.phase_tiles.append(w)
        return w

    def sb(self, name, shape, dt=F32, stack=None):
        name = self.prefix + name
        t = (stack or self.es).enter_context(self.nc.sbuf_tensor(name, list(shape), dt))
        return self._reg(t, name)

    def ps(self, name, shape, dt=F32, stack=None):
        name = self.prefix + name
        t = (stack or self.es).enter_context(self.nc.psum_tensor(name, list(shape), dt))
        w = self._reg(t, name)
        w.is_psum = True
        return w

    def dram(self, name, shape, dt=F32, kind="Internal", raw=False):
        if not raw:
            name = self.prefix + name
        t = self.nc.dram_tensor(name, list(shape), dt, kind=kind)
        return self._reg(t.ap(), name)

    def begin_phase(self, prefix):
        self.prefix = prefix
        self._saved_es = self.es
        self.es = contextlib.ExitStack()
        self.phase_tiles = []

    def end_phase(self):
        self.barrier()
        self.es.close()
        self.es = self._saved_es
        for tl in self.phase_tiles:
            for qt, (sem, cnt) in tl.dsem.items():
                self.dsem_pool.setdefault(qt, []).append((sem, cnt))
            tl.dsem = {}
        self.phase_tiles = []
        self.prefix = ""

    def inp(self, name, shape, dt=F32):
        return self.dram(name, shape, dt, kind="ExternalInput")

    def out(self, name, shape, dt=F32):
        return self.dram(name, shape, dt, kind="ExternalOutput")

    def _dsem(self, tile, qt):
        if qt not in tile.dsem:
            pool = self.dsem_pool.get(qt, [])
            if pool:
                sem, cnt = pool.pop()
            else:
                sem, cnt = self.sem_es.enter_context(self.nc.semaphore("ds_" + qt + "_" + tile.name)), 0
            tile.dsem[qt] = [sem, cnt]
        return tile.dsem[qt]

    def share_dsem(self, tiles):
        return tiles

    def _tl(self, aps):
        out = []
        for a in aps:
            if a is None:
                continue
            if isinstance(a, T):
                out.append(a)
            elif _isap(a):
                out.append(self.byname[a.tensor.name])
        return out

    def _deps(self, e, reads, writes, skip_waw=False):
        deps = {}

        def add(d):
            for sid, (sem, cnt) in d.items():
                if sid not in deps or deps[sid][1] < cnt:
                    deps[sid] = (sem, cnt)

        for tile in reads:
            add(tile.trk.w)
        for tile in writes:
            if not skip_waw:
                add(tile.trk.w)
            else:
                esids = {id(v) for v in self.esem.values()}
                add({k_: v_ for k_, v_ in tile.trk.w.items() if k_ in esids})
            add(tile.trk.r)
        engine = self.eng[e]
        wd = self.waited[e]
        own = id(self.esem[e])
        for sid, (sem, cnt) in deps.items():
            if sid == own and (e == "pe" or not self.sew):
                continue
            if wd.get(sid, 0) >= cnt:
                continue
            engine.wait_ge(sem, cnt)
            wd[sid] = cnt

    def _record(self, tok, reads, writes, accumulate=False):
        sem, cnt = tok
        for tile in reads:
            tile.trk.r[id(sem)] = (sem, cnt)
        for tile in writes:
            k = tile.trk
            if not accumulate:
                k.w = {}
            k.r = {}
            k.w[id(sem)] = (sem, cnt)

    def op(self, e, fn, ins=(), outs=(), accumulate=False):
        reads = self._tl(ins)
        writes = self._tl(outs)
        if e != "pe":
            writes = writes + [t_ for t_ in reads if t_.is_psum and t_ not in writes]
        self._deps(e, reads, writes)
        ins_ = fn(self.eng[e])
        self.ecnt[e] += 1
        ins_.then_inc(self.esem[e], 1)
        self.ninst += 1
        self._record((self.esem[e], self.ecnt[e]), reads, writes, accumulate)
        return ins_

    def dma(self, e, out_ap, in_ap, sem_tile=None, **kw):
        reads = self._tl([in_ap])
        writes = self._tl([out_ap])
        self._deps(e, reads, writes, skip_waw=True)
        if sem_tile is None:
            sem_tile = writes[0] if "SB" in type(out_ap.tensor).__name__ else reads[0]
        ent = self._dsem(sem_tile, "sw" if e == "pool" else "hw")
        sem = ent[0]
        i = self.eng[e].dma_start(out=out_ap, in_=in_ap, **kw)
        ent[1] += 16
        i.then_inc(sem, 16)
        self.dsem_latest[id(sem)] = (sem, ent[1])
        self.ninst += 1
        self._record((sem, ent[1]), reads, writes, accumulate=True)
        return i

    def barrier(self):
        for e in self.eng:
            engine = self.eng[e]
            wd = self.waited[e]
            for o in self.eng:
                if o == e:
                    continue
                c = self.ecnt[o]
                if c > wd.get(id(self.esem[o]), 0):
                    engine.wait_ge(self.esem[o], c)
                    wd[id(self.esem[o])] = c
            for sid, (sem, cnt) in self.dsem_latest.items():
                if cnt > wd.get(sid, 0):
                    engine.wait_ge(sem, cnt)
                    wd[sid] = cnt

    def finish(self):
        self.barrier()
        self.es.close()

    def mm(self, out, lhsT, rhs, start=True, stop=True):
        return self.op("pe", lambda en: en.matmul(out, lhsT, rhs, start=start, stop=stop),
                       ins=[lhsT, rhs], outs=[out], accumulate=not start)

    def tr(self, out, in_, ident):
        return self.op("pe", lambda en: en.transpose(out, in_, ident), ins=[in_, ident], outs=[out])

    def act(self, out, in_, func, bias=None, scale=1.0, accum=None):
        kw = {}
        if bias is not None:
            kw["bias"] = bias
        if accum is not None:
            kw["accum_out"] = accum
        return self.op("act", lambda en: en.activation(out=out, in_=in_, func=func, scale=scale, **kw),
                       ins=[in_, bias, scale], outs=[out, accum])

    def tt(self, e, out, in0, in1, op):
        return self.op(e, lambda en: en.tensor_tensor(out, in0, in1, op), ins=[in0, in1], outs=[out])

    def ts(self, e, out, in0, s1, s2=None, op0=ALU.mult, op1=None, accum=None):
        kw = {}
        if op1 is not None:
            kw["op1"] = op1
        if accum is not None:
            kw["accum_out"] = accum
        return self.op(e, lambda en: en.tensor_scalar(out, in0, s1, s2, op0, **kw),
                       ins=[in0, s1, s2], outs=[out, accum])

    def stt(self, out, in0, scalar, in1, op0=ALU.mult, op1=ALU.add):
        return self.op("dve", lambda en: en.scalar_tensor_tensor(out, in0, scalar, in1, op0, op1),
                       ins=[in0, scalar, in1], outs=[out])

    def cp(self, e, out, in_):
        if e == "act":
            return self.op("act", lambda en: en.copy(out, in_), ins=[in_], outs=[out])
        return self.op(e, lambda en: en.tensor_copy(out, in_), ins=[in_], outs=[out])

    def memset(self, e, ap, val):
        return self.op(e, lambda en: en.memset(ap, val), outs=[ap])

    def recip(self, out, in_):
        return self.op("dve", lambda en: en.reciprocal(out, in_), ins=[in_], outs=[out])

    def red(self, e, out, in_, op, axis=AX.X):
        return self.op(e, lambda en: en.tensor_reduce(out, in_, axis, op), ins=[in_], outs=[out])

    def scan(self, out, d0, d1, init, op0=ALU.mult, op1=ALU.add):
        return self.op("dve", lambda en: en.tensor_tensor_scan(out, d0, d1, init, op0, op1),
                       ins=[d0, d1, init], outs=[out])


def load_consts(P):
    c = {}
    if "idf_d" not in P.cache:
        P.cache["idf_d"] = P.dram("ident_f32", [128, 128], F32, kind="ExternalInput", raw=True)
        P.cache["idb_d"] = P.dram("ident_bf16", [128, 128], BF16, kind="ExternalInput", raw=True)
    c["idf_d"] = P.cache["idf_d"]; c["idb_d"] = P.cache["idb_d"]
    c["idf"] = P.sb("idf", [128, 128], F32)
    c["idb"] = P.sb("idb", [128, 128], BF16)
    P.dma("sp", c["idf"][:], c["idf_d"][:])
    P.dma("sp", c["idb"][:], c["idb_d"][:])
    return c


def norm_tile(P, xt, gb, hx, ss, rstd, eps=1e-6, junk=None):
    P.act(junk[:], xt[:], AF.Square, accum=ss[:])
    P.act(rstd[:], ss[:], AF.Sqrt, bias=None, scale=1.0 / D) if False else None
    P.ts("dve", rstd[:], ss[:], 1.0 / D, eps, op0=ALU.mult, op1=ALU.add)
    P.act(rstd[:], rstd[:], AF.Sqrt)
    P.recip(rstd[:], rstd[:])
    P.stt(hx[:], xt[:], rstd[:, 0:1], gb[:], op0=ALU.mult, op1=ALU.mult)


def build_ffn(n_exp, ff, moe, final_norm):
    P = Prog()
    xin = P.inp("xin", [S, D]); xout = P.out("xout", [S, D])
    P.begin_phase("")
    emit_ffn(P, xin, xout, n_exp, ff, moe, final_norm)
    P.finish()
    return P


def emit_ffn(P, xin, xout, n_exp, ff, moe, final_norm):
    nfc = ff // 128
    gn = P.inp("gnorm", [D])
    wg = P.inp("wg", [n_exp, D, ff])
    wu = P.inp("wu", [n_exp, D, ff])
    wd = P.inp("wd", [n_exp, ff, D])
    if moe:
        wr = P.inp("wr", [D, NE])
        br = P.inp("br", [NE])
    if final_norm:
        gf = P.inp("gfinal", [D])
    c = load_consts(P)

    ST = 1024
    NST = S // ST
    TPS = ST // 128
    gb = P.sb("gb", [128, D])
    P.dma("sp", gb[:], gn.t.partition_broadcast(128))
    if final_norm:
        gfb = P.sb("gfb", [128, D])
        P.dma("sp", gfb[:], gf.t.partition_broadcast(128))
    if moe:
        wr_sb = P.sb("wr_sb", [128, 8, NE])
        P.dma("sp", wr_sb[:], wr.t.rearrange("(c p) e -> p c e", p=128))
        brb = P.sb("brb", [128, NE])
        P.dma("sp", brb[:], br.t.partition_broadcast(128))
        gates_s = [P.sb(f"gates{i}", [128, TPS, NE]) for i in range(2)]
        hxf = P.sb("hxf", [128, D])
        hTf = P.sb("hTf", [128, 8, 128])
        lg = P.sb("lg", [128, NE]); eq = P.sb("eq", [128, NE]); l2 = P.sb("l2", [128, NE])
        m1 = P.sb("m1", [128, 1]); m2 = P.sb("m2", [128, 1]); nm1 = P.sb("nm1", [128, 1])
        ee = P.sb("ee", [128, NE]); den = P.sb("den", [128, 1])
        ps_r = P.ps("ps_r", [128, 8, 128])
        ps_l = P.ps("ps_l", [128, NE])
    xres_s = [[P.sb(f"xres{b}_{i}", [128, D]) for i in range(TPS)] for b in range(2)]
    hT_s = [P.sb(f"hT{b}", [128, 8, ST], BF16) for b in range(2)]
    hx = [P.sb(f"hx{i}", [128, D], BF16) for i in range(1)] * 2
    junk = P.sb("junk", [128, D], BF16)
    ss2 = P.sb("ss2", [128, 1]); rstd2 = P.sb("rstd2", [128, 1])
    ss = P.sb("ss", [128, 1]); rstd = P.sb("rstd", [128, 1])
    actT = P.sb("actT", [128, nfc, ST], BF16)
    wd_sb = [P.sb(f"wd_sb{i}", [128, 4, D], BF16) for i in range(2)]
    wgu = [P.sb(f"wgu{i}", [128, 2, 8, 128], BF16) for i in range(3)]
    sg = [P.sb(f"sg{i}", [128, 512], BF16) for i in range(2)]
    ps_t = [P.ps(f"ps_t{i}", [128, 8, 128], BF16) for i in range(1)]
    n_mm = 2 if moe else 3
    ps_g = [P.ps(f"ps_g{i}", [128, 512]) for i in range(n_mm)]
    ps_u = [P.ps(f"ps_u{i}", [128, 512]) for i in range(n_mm)]
    NWD = 4

    def norm_stage(st):
        xres = xres_s[st % 2]; hT = hT_s[st % 2]
        if moe:
            gates = gates_s[st % 2]
        for tt in range(TPS):
            r0 = st * ST + tt * 128
            xt = xres[tt]
            P.dma("sp", xt[:], xin[r0:r0 + 128, :])
            h = hx[tt % 2]
            norm_tile(P, xt, gb, h, ss, rstd, junk=junk)
            if moe:
                P.stt(hxf[:], xt[:], rstd[:, 0:1], gb[:], op0=ALU.mult, op1=ALU.mult)
            yield
            pt = ps_t[0]
            for dc in range(8):
                P.tr(pt[:, dc, :], h[:, dc * 128:(dc + 1) * 128], c["idb"][:])
            P.cp("act" if tt % 2 else "dve", hT[:, :, tt * 128:(tt + 1) * 128], pt[:])
            yield
            if moe:
                for dc in range(8):
                    P.tr(ps_r[:, dc, :], hxf[:, dc * 128:(dc + 1) * 128], c["idf"][:])
                P.cp("act", hTf[:], ps_r[:])
                yield
                for dc in range(8):
                    P.mm(ps_l[:], hTf[:, dc, :], wr_sb[:, dc, :], start=(dc == 0), stop=(dc == 7))
                P.tt("dve", lg[:], ps_l[:], brb[:], ALU.add)
                yield
                P.red("dve", m1[:], lg[:], ALU.max)
                P.ts("dve", eq[:], lg[:], m1[:, 0:1], None, op0=ALU.is_equal)
                P.stt(l2[:], eq[:], -1e30, lg[:], op0=ALU.mult, op1=ALU.add)
                P.red("dve", m2[:], l2[:], ALU.max)
                P.ts("dve", eq[:], lg[:], m2[:, 0:1], None, op0=ALU.is_ge)
                P.ts("dve", nm1[:], m1[:], -1.0, None, op0=ALU.mult)
                P.act(ee[:], lg[:], AF.Exp, bias=nm1[:, 0:1])
                P.tt("dve", ee[:], ee[:], eq[:], ALU.mult)
                P.red("dve", den[:], ee[:], ALU.add)
                P.recip(den[:], den[:])
                P.ts("dve", gates[:, tt, :], ee[:], den[:, 0:1], None, op0=ALU.mult)
                yield

    for _ in norm_stage(0):
        pass
    for st in range(NST):
        xres = xres_s[st % 2]; hT = hT_s[st % 2]
        if moe:
            gates = gates_s[st % 2]
        ngen = norm_stage(st + 1) if st + 1 < NST else iter(())
        for ex in range(n_exp):
            k = 0
            for fc in range(nfc):
                w = wgu[fc % 3]
                next(ngen, None)
                P.dma("pool", w[:, 0], wg.t[ex, :, fc * 128:(fc + 1) * 128].rearrange("(c p) f -> p c f", p=128))
                P.dma("pool", w[:, 1], wu.t[ex, :, fc * 128:(fc + 1) * 128].rearrange("(c p) f -> p c f", p=128))
                for tq in range(ST // 512):
                    pg = ps_g[k % n_mm]; pu = ps_u[k % n_mm]; sgt = sg[k % 2]; k += 1
                    for dc in range(8):
                        P.mm(pg[:], w[:, 0, dc, :], hT[:, dc, tq * 512:(tq + 1) * 512], start=(dc == 0), stop=(dc == 7))
                    for dc in range(8):
                        P.mm(pu[:], w[:, 1, dc, :], hT[:, dc, tq * 512:(tq + 1) * 512], start=(dc == 0), stop=(dc == 7))
                    P.act(sgt[:], pg[:], AF.Silu)
                    P.tt("dve", actT[:, fc, tq * 512:(tq + 1) * 512], sgt[:], pu[:], ALU.mult)
            ngrp = (nfc + NWD - 1) // NWD
            for gq in range(ngrp):
                f0 = gq * NWD
                nf = min(NWD, nfc - f0)
                wds = wd_sb[gq % 2]
                P.dma("pool", wds[:, 0:nf, :], wd.t[ex, f0 * 128:(f0 + nf) * 128, :].rearrange("(c p) n -> p c n", p=128))
                for tt in range(TPS):
                    for nh in range(2):
                        pd = ps_g[k % n_mm] if (k % 2 == 0) else ps_u[k % n_mm]
                        k += 1
                        for j in range(nf):
                            P.mm(pd[:], actT[:, f0 + j, tt * 128:(tt + 1) * 128], wds[:, j, nh * 512:(nh + 1) * 512],
                                 start=(j == 0), stop=(j == nf - 1))
                        xs = xres[tt][:, nh * 512:(nh + 1) * 512]
                        if moe:
                            P.stt(xs, pd[:], gates[:, tt, ex:ex + 1], xs, op0=ALU.mult, op1=ALU.add)
                        else:
                            P.tt("dve", xs, pd[:], xs, ALU.add)
        for _ in ngen:
            pass
        for tt in range(TPS):
            r0 = st * ST + tt * 128
            if final_norm:
                ho = sg[tt % 2]
                hof = hxf if moe else (P.byname.get(P.prefix + "hof0") or P.sb("hof0", [128, D]))
                norm_tile(P, xres[tt], gfb, hof, ss2, rstd2, junk=junk)
                P.dma("sp", xout[r0:r0 + 128, :], hof[:])
            else:
                P.dma("sp", xout[r0:r0 + 128, :], xres[tt][:])
    P.end_phase()


_CONST = None


def consts_np():
    global _CONST
    if _CONST is None:
        import ml_dtypes
        _CONST = {"ident_f32": np.eye(128, dtype=np.float32),
                  "ident_bf16": np.eye(128, dtype=np.float32).astype(ml_dtypes.bfloat16)}
    return _CONST


def run(P, in_maps):
    res = run_bass_kernel_spmd(P.nc, in_maps, core_ids=list(range(NCORES)))
    return res.results


def norm_transpose(P, c, src, nrows, gb, hT_of, tmp):
    xt2, hx2, junk, ss, rstd, pt = tmp
    for tt in range(nrows // 128):
        xt = xt2[tt % 2]
        h = hx2[tt % 2]
        P.dma("sp", xt[:], src[tt * 128:(tt + 1) * 128, :])
        norm_tile(P, xt, gb, h, ss, rstd, junk=junk)
        for dc in range(8):
            P.tr(pt[:, dc, :], h[:, dc * 128:(dc + 1) * 128], c["idb"][:])
        P.cp("act" if tt % 2 else "dve", hT_of(tt), pt[:])


def sin_reduced(P, out, ang, tmp):
    MAGIC = 12582912.0
    P.ts("dve", tmp, ang, 1.0 / (2 * math.pi), MAGIC, op0=ALU.mult, op1=ALU.add)
    P.ts("dve", tmp, tmp, -MAGIC, None, op0=ALU.add)
    P.stt(tmp, tmp, -2 * math.pi, ang, op0=ALU.mult, op1=ALU.add)
    P.ts("dve", tmp, tmp, math.pi, -math.pi, op0=ALU.min, op1=ALU.max)
    P.act(out, tmp, AF.Sin)


def kv_stage(P, c, mem, gmem, wkv, tmp, stack, kT, v_sb):
    gbm = P.sb("gbm", [128, D], stack=stack)
    P.dma("sp", gbm[:], gmem.t.partition_broadcast(128))
    wkv_sb = P.sb("wkv_sb", [128, 8, 512], BF16, stack=stack)
    P.dma("pool", wkv_sb[:], wkv.t.rearrange("(c p) n -> p c n", p=128))
    hTm = P.sb("hTm", [128, 8, MEM], BF16, stack=stack)
    norm_transpose(P, c, mem, MEM, gbm, lambda tt: hTm[:, :, tt * 128:(tt + 1) * 128], tmp)
    ps_kv = P.ps("ps_kv", [128, 256], stack=stack)
    for j in range(2):
        for dc in range(8):
            P.mm(ps_kv[:], wkv_sb[:, dc, j * 128:(j + 1) * 128], hTm[:, dc, :], start=(dc == 0), stop=(dc == 7))
        P.memset("dve", kT[j][:], 0.0)
        for hh in range(2):
            hs = slice(hh * 64, hh * 64 + 64)
            P.cp("dve", kT[j][hs, hh, :], ps_kv[hs, :])
    P.memset("dve", v_sb[:], 0.0)
    for mt in range(2):
        for dc in range(8):
            P.mm(ps_kv[:], hTm[:, dc, mt * 128:(mt + 1) * 128], wkv_sb[:, dc, 256:512], start=(dc == 0), stop=(dc == 7))
        for h in range(4):
            P.cp("dve", v_sb[:, mt, h, (h % 2) * 64:(h % 2) * 64 + 64], ps_kv[:, h * 64:(h + 1) * 64])


SKIP = set()


def lockstep(gens):
    gens = list(gens)
    while gens:
        for g in list(gens):
            try:
                next(g)
            except StopIteration:
                gens.remove(g)


class XAttn:
    def __init__(self, P, c, stack, banks=None, tag=""):
        self.P, self.c = P, c
        if banks is None:
            self.ps_s = [P.ps(f"ps_s{tag}{i}", [128, 2, 256], stack=stack) for i in range(2)]
            self.ps_pt = P.ps("ps_pt" + tag, [128, 8, 128], BF16, stack=stack)
            self.ps_o = P.ps("ps_o" + tag, [128, 2, 128], stack=stack)
            self.v_s = [t[:] for t in self.ps_s]
            self.v_pt = self.ps_pt[:]
            self.v_o = self.ps_o[:]
        else:
            b0, b1, b2, b3 = banks
            self.v_s = [b0[:].rearrange("p (a b) -> p a b", a=2), b1[:].rearrange("p (a b) -> p a b", a=2)]
            self.v_pt = b2[:].bitcast(BF16).rearrange("p (a b) -> p a b", a=8)
            self.v_o = b3[:, 0:256].rearrange("p (a b) -> p a b", a=2)
        sb = lambda n, sh, dt=F32: P.sb(n + tag, sh, dt, stack=stack)
        self.mx = sb("mx", [128, 4]); self.nmx = sb("nmx", [128, 4])
        self.sm = sb("sm", [128, 4]); self.rs = sb("rs", [128, 4])
        self.pexp = sb("pexp", [128, 4, 256], BF16)
        self.pn = sb("pn", [128, 4, 256], BF16)
        self.pT = sb("pT_sb", [128, 8, 128], BF16)

    def gen(self, q_of, kT, v_sb, out_ap):
        P, c = self.P, self.c
        mx, nmx, sm, rs, pexp, pn, pT = self.mx, self.nmx, self.sm, self.rs, self.pexp, self.pn, self.pT
        for h in range(4):
            P.mm(self.v_s[h // 2][:, h % 2, :], q_of(h // 2), kT[h // 2][:, h % 2, :])
        yield
        for pr in range(2):
            P.red("dve", mx[:, pr * 2:pr * 2 + 2], self.v_s[pr], ALU.max)
        P.ts("dve", nmx[:], mx[:], -0.125, None, op0=ALU.mult)
        yield
        for h in range(4):
            P.act(pexp[:, h, :], self.v_s[h // 2][:, h % 2, :], AF.Exp, bias=nmx[:, h:h + 1], scale=0.125,
                  accum=sm[:, h:h + 1])
        yield
        P.recip(rs[:], sm[:])
        for h in range(4):
            P.ts("pool" if h % 2 else "dve", pn[:, h, :], pexp[:, h, :], rs[:, h:h + 1], None, op0=ALU.mult)
        yield
        for h in range(4):
            for mt in range(2):
                P.tr(self.v_pt[:, h * 2 + mt, :], pn[:, h, mt * 128:(mt + 1) * 128], c["idb"][:])
        P.cp("act", pT[:], self.v_pt)
        yield
        for h in range(4):
            for mt in range(2):
                P.mm(self.v_o[:, h // 2, :], v_sb[:, mt, h, :], pT[:, h * 2 + mt, :],
                     start=(h % 2 == 0 and mt == 0), stop=(h % 2 == 1 and mt == 1))
        P.cp("dve", out_ap, self.v_o)
        yield

    def tile(self, q_of, kT, v_sb, out_ap):
        for _ in self.gen(q_of, kT, v_sb, out_ap):
            pass


def xattn_stage(P, c, qT, kT, v_sb, caT, stack):
    xas = [XAttn(P, c, stack, tag=f"_{i}") for i in range(2)]

    def tl(tt):
        tsl = slice(tt * 128, (tt + 1) * 128)
        return lambda pr: qT[pr][:, tsl]

    for t2 in range(0, NT, 2):
        lockstep([xas[i].gen(tl(t2 + i), kT, v_sb, caT[:, :, (t2 + i) * 128:(t2 + i + 1) * 128]) for i in range(2)])


def outproj_stage(P, x_src, xout, mcT_of, wout, stack):
    wout_sb = P.sb("wout_sb", [128, 8, D], BF16, stack=stack)
    P.dma("pool", wout_sb[:], wout.t.rearrange("(c p) n -> p c n", p=128))
    xo = [P.sb(f"xo{i}", [128, D], stack=stack) for i in range(2)]
    ps_x = [P.ps(f"ps_x{i}", [128, 512], stack=stack) for i in range(4)]
    for tt in range(NT):
        tsl = slice(tt * 128, (tt + 1) * 128)
        xt = xo[tt % 2]
        P.dma("sp", xt[:], x_src[tsl, :])
        for nh in range(2):
            pp = ps_x[(tt % 2) * 2 + nh]
            for kc in range(8):
                P.mm(pp[:], mcT_of(kc, tsl), wout_sb[:, kc, nh * 512:(nh + 1) * 512], start=(kc == 0), stop=(kc == 7))
            P.tt("dve", xt[:, nh * 512:(nh + 1) * 512], pp[:], xt[:, nh * 512:(nh + 1) * 512], ALU.add)
        P.dma("sp", xout[tsl, :], xt[:])


def build_l0a(stop_after=99):
    P = Prog()
    x = P.inp("x", [S, D]); mem = P.inp("mem", [MEM, D]); xout = P.out("xout", [S, D])
    P.begin_phase("")
    emit_l0a(P, x, mem, xout)
    P.finish()
    return P


def emit_l0a(P, x, mem, xout, stop_after=99):
    gmix = P.inp("gmix", [D]); gmem = P.inp("gmem", [D])
    wkv = P.inp("wkv", [D, 512]); win = P.inp("win", [D, D]); wout = P.inp("wout", [D, D])
    lam_re = P.inp("lam_re", [48, 64]); lam_im = P.inp("lam_im", [48, 64]); log_dt = P.inp("log_dt", [48])
    b_re = P.inp("b_re", [48, 64, 16]); b_im = P.inp("b_im", [48, 64, 16])
    c_re = P.inp("c_re", [48, 16, 64]); c_im = P.inp("c_im", [48, 16, 64])
    dsk = P.inp("dsk", [768]); wglu = P.inp("wglu", [48, 16, 16]); bglu = P.inp("bglu", [768])
    c = load_consts(P)
    uT = [P.sb(f"uT{i}", [128, S], BF16) for i in range(6)]
    caT = P.sb("caT", [128, 2, S], BF16)
    kT = [P.sb(f"kT{j}", [128, 2, MEM], BF16) for j in range(2)]
    v_sb = P.sb("v_sb", [128, 2, 4, 128], BF16)
    stq = contextlib.ExitStack()
    qT = [P.sb(f"qT{i}", [128, S], BF16, stack=stq) for i in range(2)]

    st1 = contextlib.ExitStack()
    xt2 = [P.sb(f"xt{i}", [128, D], stack=st1) for i in range(2)]
    hx2 = [P.sb(f"hxb{i}", [128, D], BF16, stack=st1) for i in range(2)]
    junk = P.sb("junk", [128, D], stack=st1)
    ss = P.sb("ss", [128, 1], stack=st1); rstd = P.sb("rstd", [128, 1], stack=st1)
    pt = P.ps("pt", [128, 8, 128], BF16, stack=st1)
    tmp = (xt2, hx2, junk, ss, rstd, pt)
    kv_stage(P, c, mem, gmem, wkv, tmp, st1, kT, v_sb)
    gb = P.sb("gb", [128, D], stack=st1)
    P.dma("sp", gb[:], gmix.t.partition_broadcast(128))
    win_sb = P.sb("win_sb", [128, 8, D], BF16, stack=st1)
    P.dma("pool", win_sb[:], win.t.rearrange("(c p) n -> p c n", p=128))
    hT = [P.sb(f"hT{i}", [128, 8, 512], BF16, stack=st1) for i in range(NTC)]
    norm_transpose(P, c, x, S, gb, lambda tt: hT[tt // 4][:, :, (tt % 4) * 128:(tt % 4 + 1) * 128], tmp)
    ps_p = [P.ps(f"ps_p{i}", [128, 512], stack=st1) for i in range(4)]
    k = 0
    for tc in range(NTC):
        for ct in range(8):
            pp = ps_p[k % 4]; k += 1
            for dc in range(8):
                P.mm(pp[:], win_sb[:, dc, ct * 128:(ct + 1) * 128], hT[tc][:, dc, :], start=(dc == 0), stop=(dc == 7))
            dst = uT[ct] if ct < 6 else qT[ct - 6]
            P.cp("act" if k % 2 else "dve", dst[:, tc * 512:(tc + 1) * 512], pp[:])
    P.barrier()
    st1.close()

    st2 = contextlib.ExitStack()
    xattn_stage(P, c, qT, kT, v_sb, caT, st2)
    P.barrier()
    st2.close()
    stq.close()

    st3 = contextlib.ExitStack()
    s5_stage(P, c, uT, lam_re, lam_im, log_dt, b_re, b_im, c_re, c_im, dsk, wglu, bglu, st3)
    P.barrier()
    st3.close()

    st4 = contextlib.ExitStack()
    outproj_stage(P, x, xout, lambda kc, tsl: (uT[kc][:, tsl] if kc < 6 else caT[:, kc - 6, tsl]), wout, st4)
    P.barrier()
    st4.close()
    P.end_phase()


def s5_stage(P, c, uT, lam_re, lam_im, log_dt, b_re, b_im, c_re, c_im, dsk, wglu, bglu, st):
    NJ = 24
    nc = P.nc

    pw_re = P.sb("s5_pwre", [128, NJ, 12], stack=st); pw_im = P.sb("s5_pwim", [128, NJ, 12], stack=st)
    pw_imn = P.sb("s5_pwimn", [128, NJ, 12], stack=st)
    WB = [P.sb("s5_WBre", [128, NJ, 128], BF16, stack=st), P.sb("s5_WBim", [128, NJ, 128], BF16, stack=st)]
    WC = [P.sb("s5_WCre", [128, NJ, 128], BF16, stack=st), P.sb("s5_WCim", [128, NJ, 128], BF16, stack=st)]
    glw = P.sb("s5_glw", [128, 6, 128], BF16, stack=st)
    dcol = P.sb("s5_d", [128, 6], stack=st); bgl = P.sb("s5_bgl", [128, 6], stack=st)
    stp = contextlib.ExitStack()
    cur = [stp]

    def sbt(name, shape, dt=F32):
        return P.sb(name, shape, dt, stack=cur[0])

    lr = sbt("s5_lr", [128, NJ]); li = sbt("s5_li", [128, NJ]); dt = sbt("s5_dt", [128, NJ])
    with nc.allow_non_contiguous_dma(reason="tiny param loads"):
        for gi in range(2):
            hs = slice(gi * 64, gi * 64 + 64)
            P.dma("sp", lr[hs, :], lam_re.t.rearrange("(j g) p -> g p j", g=2)[gi])
            P.dma("sp", li[hs, :], lam_im.t.rearrange("(j g) p -> g p j", g=2)[gi])
            P.dma("sp", dt[hs, :], log_dt.t.rearrange("(j g) -> g j", g=2)[gi].partition_broadcast(64))
    P.act(dt[:], dt[:], AF.Exp)
    t0 = sbt("s5_t0", [128, NJ]); t1 = sbt("s5_t1", [128, NJ]); t2 = sbt("s5_t2", [128, NJ])
    mag = sbt("s5_mag", [128, NJ]); ang = sbt("s5_ang", [128, NJ])
    sn = sbt("s5_sn", [128, NJ]); cs = sbt("s5_cs", [128, NJ])
    P.tt("dve", t0[:], lr[:], dt[:], ALU.mult)
    P.act(mag[:], t0[:], AF.Exp)
    P.tt("dve", ang[:], li[:], dt[:], ALU.mult)
    sin_reduced(P, sn[:], ang[:], t1[:])
    P.ts("dve", t2[:], ang[:], math.pi / 2, None, op0=ALU.add)
    sin_reduced(P, cs[:], t2[:], t1[:])
    P.tt("dve", pw_re[:, :, 0], mag[:], cs[:], ALU.mult)
    P.tt("dve", pw_im[:, :, 0], mag[:], sn[:], ALU.mult)
    for k in range(1, 12):
        ar = pw_re[:, :, k - 1]; ai = pw_im[:, :, k - 1]
        P.tt("dve", t0[:], ar, ar, ALU.mult)
        P.tt("dve", t1[:], ai, ai, ALU.mult)
        P.tt("dve", pw_re[:, :, k], t0[:], t1[:], ALU.subtract)
        P.tt("dve", t0[:], ar, ai, ALU.mult)
        P.ts("dve", pw_im[:, :, k], t0[:], 2.0, None, op0=ALU.mult)
    P.ts("dve", pw_imn[:], pw_im[:], -1.0, None, op0=ALU.mult)
    zre = sbt("s5_zre", [128, NJ, 1]); zim = sbt("s5_zim", [128, NJ, 1]); den = sbt("s5_den", [128, NJ])
    nre = sbt("s5_nre", [128, NJ])
    P.ts("dve", nre[:], pw_re[:, :, 0], -1.0, None, op0=ALU.add)
    nim = pw_im[:, :, 0]
    P.tt("dve", t0[:], lr[:], lr[:], ALU.mult)
    P.tt("dve", t1[:], li[:], li[:], ALU.mult)
    P.tt("dve", den[:], t0[:], t1[:], ALU.add)
    P.recip(den[:], den[:])
    P.tt("dve", t0[:], nre[:], lr[:], ALU.mult)
    P.tt("dve", t1[:], nim, li[:], ALU.mult)
    P.tt("dve", t0[:], t0[:], t1[:], ALU.add)
    P.tt("dve", zre[:, :, 0], t0[:], den[:], ALU.mult)
    P.tt("dve", t0[:], nim, lr[:], ALU.mult)
    P.tt("dve", t1[:], nre[:], li[:], ALU.mult)
    P.tt("dve", t0[:], t0[:], t1[:], ALU.subtract)
    P.tt("dve", zim[:, :, 0], t0[:], den[:], ALU.mult)
    braw_re = sbt("s5_brre", [128, NJ, 16]); braw_im = sbt("s5_brim", [128, NJ, 16])
    for gi in range(2):
        hs = slice(gi * 64, gi * 64 + 64)
        P.dma("sp", braw_re[hs, :, :], b_re.t.rearrange("(j g) p i -> g p j i", g=2)[gi])
        P.dma("sp", braw_im[hs, :, :], b_im.t.rearrange("(j g) p i -> g p j i", g=2)[gi])
    bb_re = sbt("s5_bbre", [128, NJ, 16]); bb_im = sbt("s5_bbim", [128, NJ, 16]); bt = sbt("s5_bt", [128, NJ, 16])
    zreb = zre[:].to_broadcast([128, NJ, 16]); zimb = zim[:].to_broadcast([128, NJ, 16])
    P.tt("dve", bb_re[:], braw_re[:], zreb, ALU.mult)
    P.tt("dve", bt[:], braw_im[:], zimb, ALU.mult)
    P.tt("dve", bb_re[:], bb_re[:], bt[:], ALU.subtract)
    P.tt("dve", bb_im[:], braw_im[:], zreb, ALU.mult)
    P.tt("dve", bt[:], braw_re[:], zimb, ALU.mult)
    P.tt("dve", bb_im[:], bb_im[:], bt[:], ALU.add)
    pad = sbt("s5_pad", [128, NJ, 128])
    ps_w = [P.ps(f"s5_psw{i}", [128, 4, 128], stack=stp) for i in range(2)]
    for ri, bb in enumerate((bb_re, bb_im)):
        P.memset("pool", pad[:], 0.0)
        padv = pad[:].rearrange("p (ct jq) c -> p ct jq c", jq=4)
        bbv = bb[:].rearrange("p (ct jq) i -> p ct jq i", jq=4)
        for gi in range(2):
            hs = slice(gi * 64, gi * 64 + 64)
            for jq in range(4):
                c0 = jq * 32 + gi * 16
                P.cp("pool", padv[hs, :, jq, c0:c0 + 16], bbv[hs, :, jq, :])
        for j4 in range(NJ // 4):
            pw = ps_w[j4 % 2]
            for q in range(4):
                P.tr(pw[:, q, :], pad[:, j4 * 4 + q, :], c["idf"][:])
            P.cp("act", WB[ri][:, j4 * 4:(j4 + 1) * 4, :], pw[:])
    for ri, cc in enumerate((c_re, c_im)):
        P.memset("pool", pad[:], 0.0)
        padv = pad[:].rearrange("p (ct jq) c -> p ct jq c", jq=4)
        for gi in range(2):
            for jq in range(4):
                p0 = jq * 32 + gi * 16
                src = cc.t.rearrange("(ct jq g) j p -> jq g j ct p", jq=4, g=2)[jq, gi]
                P.dma("sp", padv[p0:p0 + 16, :, jq, gi * 64:gi * 64 + 64], src)
        for j4 in range(NJ // 4):
            pw = ps_w[j4 % 2]
            for q in range(4):
                P.tr(pw[:, q, :], pad[:, j4 * 4 + q, :], c["idf"][:])
            if ri == 0:
                P.cp("act", WC[ri][:, j4 * 4:(j4 + 1) * 4, :], pw[:])
            else:
                P.act(WC[ri][:, j4 * 4:(j4 + 1) * 4, :], pw[:], AF.Copy, scale=-1.0)
    gl32 = sbt("s5_gl32", [128, 6, 128])
    P.memset("pool", gl32[:], 0.0)
    for g_l in range(8):
        src = wglu.t.rearrange("(ct gl) j k -> gl j ct k", gl=8)[g_l]
        P.dma("sp", gl32[g_l * 16:(g_l + 1) * 16, :, g_l * 16:(g_l + 1) * 16], src)
    P.cp("dve", glw[:], gl32[:])
    with nc.allow_non_contiguous_dma(reason="tiny param loads"):
        P.dma("sp", dcol[:], dsk.t.rearrange("(ct p) -> p ct", p=128))
        P.dma("sp", bgl[:], bglu.t.rearrange("(ct p) -> p ct", p=128))

    P.barrier()
    stp.close()
    cur[0] = st
    xr = [sbt(f"s5_xr{i}", [128, S]) for i in range(2)]
    xi = [sbt(f"s5_xi{i}", [128, S]) for i in range(2)]
    xr16 = sbt("s5_xr16", [128, S], BF16); xi16 = sbt("s5_xi16", [128, S], BF16)
    yacc = sbt("s5_yacc", [128, S])
    ps_b = [P.ps(f"s5_psb{i}", [128, 512], stack=st) for i in range(4)]
    ps_g = [P.ps(f"s5_psg{i}", [128, 512], stack=st) for i in range(2)]
    gy = [sbt(f"s5_gy{i}", [128, 512]) for i in range(1)] * 2
    g2 = [sbt(f"s5_g2{i}", [128, 512]) for i in range(1)] * 2
    gt = [sbt(f"s5_gt{i}", [128, 512]) for i in range(1)] * 2
    ge = [sbt(f"s5_ge{i}", [128, 512]) for i in range(1)] * 2
    ge16 = [sbt(f"s5_ge16{i}", [128, 512], BF16) for i in range(2)]
    sgl = [sbt(f"s5_sgl{i}", [128, 512]) for i in range(1)] * 2
    kkc = [0]

    def st_bu(jt):
        ct = jt // 4
        X = (xr[jt % 2], xi[jt % 2])
        for tc in range(NTC):
            tsl = slice(tc * 512, (tc + 1) * 512)
            for ri in range(2):
                pb = ps_b[kkc[0] % 4]; kkc[0] += 1
                P.mm(pb[:], WB[ri][:, jt, :], uT[ct][:, tsl])
                P.cp("act", X[ri][:, tsl], pb[:])

    def st_scan(jt):
        X = (xr[jt % 2], xi[jt % 2])
        for phase in range(2):
            ks = range(12) if phase == 0 else range(10, -1, -1)
            for k in ks:
                d = 1 << k
                vr = X[0][:].rearrange("p (n s) -> p n s", s=2 * d)
                vi = X[1][:].rearrange("p (n s) -> p n s", s=2 * d)
                if phase == 0:
                    dr, di = vr[:, :, 2 * d - 1], vi[:, :, 2 * d - 1]
                    sr, si = vr[:, :, d - 1], vi[:, :, d - 1]
                else:
                    dr, di = vr[:, 1:, d - 1], vi[:, 1:, d - 1]
                    sr, si = vr[:, :-1, 2 * d - 1], vi[:, :-1, 2 * d - 1]
                ar = pw_re[:, jt, k:k + 1]; ai = pw_im[:, jt, k:k + 1]; ain = pw_imn[:, jt, k:k + 1]
                P.stt(dr, sr, ar, dr)
                P.stt(dr, si, ain, dr)
                P.stt(di, si, ar, di)
                P.stt(di, sr, ai, di)

    def st_cast(jt):
        X = (xr[jt % 2], xi[jt % 2])
        P.cp("act", xr16[:], X[0][:])
        P.cp("pool", xi16[:], X[1][:])

    def st_cmm(jt):
        jq = jt % 4
        for tc in range(NTC):
            tsl = slice(tc * 512, (tc + 1) * 512)
            pb = ps_b[kkc[0] % 4]; kkc[0] += 1
            P.mm(pb[:], WC[0][:, jt, :], xr16[:, tsl], start=True, stop=False)
            P.mm(pb[:], WC[1][:, jt, :], xi16[:, tsl], start=False, stop=True)
            if jq == 0:
                P.cp("dve", yacc[:, tsl], pb[:])
            else:
                P.tt("dve", yacc[:, tsl], pb[:], yacc[:, tsl], ALU.add)

    def st_epi(ct):
        for tc in range(NTC):
            tsl = slice(tc * 512, (tc + 1) * 512)
            i2 = tc % 2
            P.stt(gy[i2][:], uT[ct][:, tsl], dcol[:, ct:ct + 1], yacc[:, tsl])
            P.tt("pool", g2[i2][:], gy[i2][:], gy[i2][:], ALU.mult)
            P.ts("pool", g2[i2][:], g2[i2][:], 0.044715, 1.0, op0=ALU.mult, op1=ALU.add)
            P.tt("pool", gt[i2][:], g2[i2][:], gy[i2][:], ALU.mult)
            P.act(gt[i2][:], gt[i2][:], AF.Sigmoid, scale=1.5957691216057308)
            P.tt("pool", ge[i2][:], gt[i2][:], gy[i2][:], ALU.mult)
            P.cp("pool", ge16[i2][:], ge[i2][:])
            P.mm(ps_g[i2][:], glw[:, ct, :], ge16[i2][:])
            P.act(sgl[i2][:], ps_g[i2][:], AF.Sigmoid, bias=bgl[:, ct:ct + 1])
            P.tt("pool", uT[ct][:, tsl], ge[i2][:], sgl[i2][:], ALU.mult)

    st_bu(0)
    for jt in range(NJ):
        if jt + 1 < NJ:
            st_bu(jt + 1)
        st_scan(jt)
        if jt >= 1:
            st_cmm(jt - 1)
            if (jt - 1) % 4 == 3:
                st_epi((jt - 1) // 4)
        st_cast(jt)
    st_cmm(NJ - 1)
    st_epi(5)


def rwkv_consts_np():
    s = np.arange(64)[:, None]; t = np.arange(64)[None, :]
    strict = (s < t).astype(np.float32); incl = (s <= t).astype(np.float32)
    maskA = np.concatenate([strict, incl, strict, incl, (t < s).astype(np.float32)], axis=1)
    maskZ = np.zeros((64, 2, 128), np.float32); maskZ[:, 0, 0:64] = 1; maskZ[:, 1, 64:128] = 1
    ones_bd = np.zeros((128, 128), np.float32); ones_bd[0:64, 0:64] = 1; ones_bd[64:, 64:] = 1
    seg = np.ones((128, 512), np.float32); seg[:, ::64] = 0
    return {"maskA": maskA, "maskZ": maskZ, "ones_bd": ones_bd, "segmask": seg}


def build_l1a():
    P = Prog()
    x = P.inp("x", [S, D]); mem = P.inp("mem", [MEM, D]); xout = P.out("xout", [S, D])
    P.begin_phase("")
    emit_l1a(P, x, mem, xout)
    P.finish()
    return P


def emit_l1a(P, x, mem, xout):
    nc = P.nc
    gmix = P.inp("gmix", [D]); gmem = P.inp("gmem", [D])
    wkv = P.inp("wkv", [D, 512]); win = P.inp("win", [D, 2816]); wout = P.inp("wout", [D, D])
    mu = P.inp("mu", [2560]); w0 = P.inp("w0", [768]); w2 = P.inp("w2", [64, 768]); a0 = P.inp("a0", [768])
    a2 = P.inp("a2", [64, 768]); g2 = P.inp("g2", [128, 768]); k_k = P.inp("k_k", [768]); k_a = P.inp("k_a", [768])
    r_k = P.inp("r_k", [768]); lnw = P.inp("lnw", [768]); lnb = P.inp("lnb", [768])
    maskA_d = P.inp("maskA", [64, 320]); maskZ_d = P.inp("maskZ", [64, 2, 128])
    ones_d = P.inp("ones_bd", [128, 128]); seg_d = P.inp("segmask", [128, 512])
    c = load_consts(P)
    kT = [P.sb(f"kT{j}", [128, 2, MEM], BF16) for j in range(2)]
    v_sb = P.sb("v_sb", [128, 2, 4, 128], BF16)
    st1 = contextlib.ExitStack()
    xt2 = [P.sb(f"xt{i}", [128, D], stack=st1) for i in range(2)]
    hx2 = [P.sb(f"hxb{i}", [128, D], BF16, stack=st1) for i in range(2)]
    junk = P.sb("junk", [128, D], stack=st1)
    ss = P.sb("ss", [128, 1], stack=st1); rstd = P.sb("rstd", [128, 1], stack=st1)
    pt = P.ps("pt", [128, 8, 128], BF16, stack=st1)
    kv_stage(P, c, mem, gmem, wkv, (xt2, hx2, junk, ss, rstd, pt), st1, kT, v_sb)
    P.barrier()
    st1.close()

    sb = P.sb
    B = [P.ps(f"bank{i}", [128, 512]) for i in range(8)]
    wbuf = [sb(f"wbuf{i}", [128, 8, 128], BF16) for i in range(3)]
    wob = [sb(f"wob{i}", [128, 512], BF16) for i in range(3)]
    gb = sb("gb", [128, D]); P.dma("sp", gb[:], gmix.t.partition_broadcast(128))
    lora = sb("lora", [128, 2, 768], BF16)
    P.memset("pool", lora[:], 0.0)
    P.dma("pool", lora[0:64, 0, :], w2[:]); P.dma("pool", lora[64:128, 1, :], a2[:])
    g2_sb = sb("g2_sb", [128, 768], BF16); P.dma("pool", g2_sb[:], g2[:])
    maskA = sb("maskA_sb", [64, 320]); P.dma("sp", maskA[:], maskA_d[:])
    maskZ = sb("maskZ_sb", [64, 2, 128]); P.dma("sp", maskZ[:], maskZ_d[:])
    ones_bd = sb("ones_sb", [128, 128]); P.dma("sp", ones_bd[:], ones_d[:])
    seg = sb("seg_sb", [128, 512]); P.dma("sp", seg[:], seg_d[:])
    prm = {}
    with nc.allow_non_contiguous_dma(reason="tiny param loads"):
        for nm, tns in (("w0", w0), ("a0", a0), ("k_k", k_k), ("k_a", k_a), ("r_k", r_k), ("lnw", lnw), ("lnb", lnb)):
            prm[nm] = sb("prm_" + nm, [128, 6])
            P.dma("sp", prm[nm][:], tns.t.rearrange("(c p) -> p c", p=128))
        mu_sb = sb("mu_sb", [128, 20])
        P.dma("sp", mu_sb[:], mu.t.rearrange("(c p) -> p c", p=128))
    Zbd = [sb(f"Zbd{i}", [128, 128]) for i in range(6)]
    Zb16 = [sb(f"Zb16_{i}", [128, 128], BF16) for i in range(6)]
    for z in Zbd + Zb16:
        P.memset("pool", z[:], 0.0)
    praw = sb("praw", [128, 20, 513])
    P.memset("pool", praw[:, :, 0:1], 0.0)
    carry = sb("carry", [128, 20, 1])
    hT = sb("hT", [128, 8, 512], BF16)
    xt = [sb(f"xl{i}", [128, D]) for i in range(2)]
    hx = [sb(f"hxl{i}", [128, D], BF16) for i in range(1)] * 2
    ss = sb("ssl", [128, 1]); rstd = sb("rstdl", [128, 1])
    qb = sb("qb", [128, 2, 512], BF16); caT = sb("caTc", [128, 2, 512], BF16)

    class _J:
        def __getitem__(self, idx):
            return qb[:].rearrange("p a b -> p (a b)")
    junk = _J()
    mixT = hT
    tw = sb("tw", [128, 512], BF16); sgd = sb("sgd", [128, 512], BF16)
    xas = [XAttn(P, c, P.es, banks=(B[1], B[2], B[0], B[3]), tag="_0"),
           XAttn(P, c, P.es, banks=(B[5], B[6], B[4], B[7]), tag="_1")]
    G = 3
    AS = []
    for i in range(G):
        A_ = {n: sb(f"rk{i}_" + n, [128, 512]) for n in
              ("lw", "a", "kk", "t1", "t2", "cl", "ecl", "encl", "eclp")}
        A_["yc"] = A_["kk"]
        A_["kkn"] = A_["kk"]
        AS.append(A_)
    tmp = AS[0]["t1"]
    sets = []
    for i in range(G):
        d = {}
        d["yT"] = sb(f"rk{i}_yT", [128, 512])
        for n in ("BT", "KT", "v16", "kmod", "g"):
            d[n] = sb(f"rk{i}_{n}", [128, 512], BF16)
        d["AR"] = sb(f"rk{i}_AR", [128, 8, 128], BF16)
        d["gam"] = sb(f"rk{i}_gam", [128, 8])
        zl = []
        for n in ("Vz", "Bz", "Kz", "Uz"):
            d[n] = sb(f"rk{i}_{n}", [128, 2, 128], BF16); zl.append(d[n])
        d["AM"] = [sb(f"rk{i}_AM{h}", [128, 320], BF16) for h in range(2)]
        d["Qp"] = [sb(f"rk{i}_Qp{j}", [128, 2, 64], BF16) for j in range(2)]
        d["Qn"] = [sb(f"rk{i}_Qn{j}", [128, 2, 64], BF16) for j in range(2)]
        d["Pp"] = sb(f"rk{i}_Pp", [128, 2, 64], BF16); d["T1"] = sb(f"rk{i}_T1", [128, 128], BF16)
        d["tmpZ"] = sb(f"rk{i}_tmpZ", [128, 64])
        zl += d["AM"] + d["Qp"] + d["Qn"] + [d["Pp"], d["T1"]]
        for n in ("BZc", "KZc", "AZc"):
            d[n] = sb(f"rk{i}_{n}", [128, 2, 64], BF16); zl.append(d[n])
        for z in zl:
            P.memset("pool", z[:], 0.0)
        d["bk"] = (B[i], B[i]) if G == 6 else (B[2 * i], B[2 * i + 1])
        d["A"] = AS[i]
        sets.append(d)
    EXPN = -math.exp(-0.5)

    def c3(ap):
        return ap.rearrange("p (c t) -> p c t", t=64)

    def prep(hp, d):
        A = d["A"]
        r_ = praw[:, hp, 1:513]; k_ = praw[:, 6 + hp, 1:513]
        cs = slice(hp * 128, (hp + 1) * 128)
        pc = lambda n: prm[n][:, hp:hp + 1]
        pb0, pb1 = d["bk"]
        P.mm(pb0[:], lora[:, 0, cs], tw[:])
        P.act(A["lw"][:], pb0[:], AF.Sigmoid, bias=pc("w0"))
        P.mm(pb1[:], lora[:, 1, cs], tw[:])
        P.act(A["a"][:], pb1[:], AF.Sigmoid, bias=pc("a0"))
        P.ts("dve", A["kk"][:], k_, pc("k_k"), None, op0=ALU.mult)
        yield
        P.ts("pool", A["lw"][:], A["lw"][:], EXPN, None, op0=ALU.mult)
        P.mm(pb0[:], g2_sb[:, cs], sgd[:])
        P.cp("act", d["g"][:], pb0[:])
        P.tt("pool", A["t1"][:], A["kk"][:], A["kk"][:], ALU.mult)
        yield
        P.scan(A["cl"][:], seg[:], A["lw"][:], 0.0)
        P.mm(pb1[:], ones_bd[:], A["t1"][:])
        P.ts("dve", A["t2"][:], pb1[:], 1e-24, None, op0=ALU.max)
        yield
        P.act(A["t2"][:], A["t2"][:], AF.Sqrt)
        P.ts("dve", A["t1"][:], A["a"][:], -1.0, pc("k_a"), op0=ALU.add, op1=ALU.mult)
        yield
        P.recip(A["t2"][:], A["t2"][:])
        P.act(A["ecl"][:], A["cl"][:], AF.Exp)
        P.act(A["encl"][:], A["cl"][:], AF.Exp, scale=-1.0)
        yield
        P.tt("pool", A["kkn"][:], A["kk"][:], A["t2"][:], ALU.mult)
        P.stt(d["kmod"][:], A["t1"][:], 1.0, k_, op0=ALU.add, op1=ALU.mult)
        yield
        P.tt("pool", A["t2"][:], A["cl"][:], A["lw"][:], ALU.subtract)
        P.tt("dve", d["AR"][:, :, 64:128], c3(r_), c3(A["ecl"][:]), ALU.mult)
        yield
        P.act(A["eclp"][:], A["t2"][:], AF.Exp)
        P.tt("pool", A["t1"][:], A["kkn"][:], A["a"][:], ALU.mult)
        P.tt("dve", d["KT"][:], d["kmod"][:], A["encl"][:], ALU.mult)
        yield
        P.stt(d["AR"][:, :, 0:64], c3(A["kkn"][:]), -1.0, c3(A["eclp"][:]), op0=ALU.mult, op1=ALU.mult)
        P.tt("pool", d["BT"][:], A["t1"][:], A["encl"][:], ALU.mult)
        P.cp("pool", d["gam"][:], c3(A["ecl"][:])[:, :, 63])
        P.cp("act", d["v16"][:], praw[:, 12 + hp, 1:513])
        yield

    def core(hp, cc, d):
        bkA, bkB = d["bk"]
        AR, BT, KT, AM, Pp, T1, tmpZ = d["AR"], d["BT"], d["KT"], d["AM"], d["Pp"], d["T1"], d["tmpZ"]
        Vz, Bz, Kz, Uz, Qp, Qn = d["Vz"], d["Bz"], d["Kz"], d["Uz"], d["Qp"], d["Qn"]
        csl = slice(cc * 64, (cc + 1) * 64)
        bT0 = bkA[0:64, 0:192].bitcast(BF16).rearrange("p (a b) -> p a b", a=3)
        bI = bkA[0:64, 0:256].rearrange("p (a b) -> p a b", a=4)
        bJ = bkB[0:64, 0:128].rearrange("p (a b) -> p a b", a=2)
        for h in range(2):
            hs = slice(h * 64, h * 64 + 64)
            P.cp("pool", d["BZc"][hs, h, :], BT[hs, csl])
            P.cp("act", d["KZc"][hs, h, :], KT[hs, csl])
            P.cp("pool", d["AZc"][hs, h, :], AR[hs, cc, 0:64])
        P.tr(bT0[:, 0, :], d["v16"][:, csl], c["idb"][:])
        P.tr(bT0[:, 1, :], BT[:, csl], c["idb"][:])
        P.tr(bT0[:, 2, :], KT[:, csl], c["idb"][:])
        for i, Xz in enumerate((Vz, Bz, Kz)):
            P.tt("dve", Xz[0:64], bT0[:, i, :].unsqueeze(1).to_broadcast([64, 2, 128]), maskZ[:], ALU.mult)
        yield
        for h in range(2):
            bA = bkB if h == 0 else bkA
            P.mm(bA[0:64, 0:128], d["BZc"][:, h, :], AR[:, cc, :])
            P.mm(bA[0:64, 128:256], d["KZc"][:, h, :], AR[:, cc, :])
            P.mm(bA[0:64, 256:320], d["AZc"][:, h, :], BT[:, csl])
            P.tt("dve", AM[h][0:64, :], bA[0:64, 0:320], maskA[:], ALU.mult)
        yield
        for h in range(2):
            P.tt("pool", Pp[0:64, h, :], AM[h][0:64, 0:64], c["idf"][0:64, 0:64], ALU.add)
        def q_step(lvl):
            o, n = (lvl - 1) % 2, lvl % 2
            for h in range(2):
                qn_o = AM[h][:, 256:320] if lvl == 1 else Qn[o][:, h, :]
                qp_o = AM[h][:, 0:64] if lvl == 1 else Qp[o][:, h, :]
                if lvl < 5:
                    P.mm(bI[:, h, :], qn_o, qp_o)
                P.mm(bI[:, 2 + h, :], qp_o, qn_o)
            if lvl < 5:
                P.cp("act", Qp[n][0:64], bI[:, 0:2, :])
            P.cp("act" if lvl % 2 else "dve", Qn[n][0:64], bI[:, 2:4, :])

        def p_step(lvl):
            n = lvl % 2
            for h in range(2):
                P.mm(bJ[:, h, :], Qn[n][:, h, :], Pp[:, h, :])
            P.tt("dve", Pp[0:64], bJ, Pp[0:64], ALU.add)

        q_step(1)
        yield
        for lvl in range(1, 6):
            p_step(lvl)
            if lvl < 5:
                q_step(lvl + 1)
            yield
        P.mm(bkA[0:64, 0:128], AR[:, cc, 0:64], Zb16[hp][:], start=True, stop=False)
        for h in range(2):
            P.mm(bkA[0:64, h * 64:(h + 1) * 64], AM[h][:, 128:192], Vz[:, h, h * 64:(h + 1) * 64],
                 start=False, stop=(h == 1))
        P.cp("act", T1[0:64], bkA[0:64, 0:128])
        yield
        for h in range(2):
            P.mm(bkB[0:64, h * 64:(h + 1) * 64], Pp[:, h, :], T1[:, h * 64:(h + 1) * 64])
        P.tt("dve", Uz[0:64], bkB[0:64, 0:128].unsqueeze(1).to_broadcast([64, 2, 128]), maskZ[:], ALU.mult)
        yield
        P.mm(bkA[:, 0:64], Zb16[hp][:], AR[:, cc, 64:128], start=True, stop=False)
        for h in range(2):
            P.mm(bkA[:, 0:64], Uz[:, h, :], AM[h][:, 64:128], start=False, stop=False)
        for h in range(2):
            P.mm(bkA[:, 0:64], Vz[:, h, :], AM[h][:, 192:256], start=False, stop=(h == 1))
        P.cp("act", d["yT"][:, csl], bkA[:, 0:64])
        yield
        for h in range(2):
            P.mm(bkB[:, 0:64], Bz[:, h, :], Uz[:, h, h * 64:(h + 1) * 64], start=(h == 0), stop=False)
        for h in range(2):
            P.mm(bkB[:, 0:64], Kz[:, h, :], Vz[:, h, h * 64:(h + 1) * 64], start=False, stop=(h == 1))
        P.act(tmpZ[:], bkB[:, 0:64], AF.Copy, scale=d["gam"][:, cc:cc + 1])
        for h in range(2):
            hs = slice(h * 64, h * 64 + 64)
            zs = Zbd[hp][hs, h * 64:(h + 1) * 64]
            P.stt(zs, zs, d["gam"][hs, cc:cc + 1], tmpZ[hs, :])
        P.cp("pool", Zb16[hp][:], Zbd[hp][:])
        yield

    def post(hp, d):
        A = d["A"]
        r_ = praw[:, hp, 1:513]; v_ = praw[:, 12 + hp, 1:513]
        pc = lambda n: prm[n][:, hp:hp + 1]
        pb0, pb1 = d["bk"]
        yT = d["yT"]
        P.mm(pb0[:], ones_bd[:], yT[:])
        P.stt(A["t1"][:], r_, pc("r_k"), d["kmod"][:], op0=ALU.mult, op1=ALU.mult)
        yield
        P.stt(A["yc"][:], pb0[:], -1.0 / 64, yT[:])
        P.mm(pb1[:], ones_bd[:], A["t1"][:])
        yield
        P.tt("pool", A["t1"][:], A["yc"][:], A["yc"][:], ALU.mult)
        P.tt("dve", A["lw"][:], pb1[:], v_, ALU.mult)
        yield
        P.mm(pb0[:], ones_bd[:], A["t1"][:])
        P.ts("dve", A["t2"][:], pb0[:], 1.0 / 64, 64e-5, op0=ALU.mult, op1=ALU.add)
        yield
        P.act(A["t2"][:], A["t2"][:], AF.Sqrt)
        yield
        P.recip(A["t2"][:], A["t2"][:])
        yield
        P.tt("pool", A["yc"][:], A["yc"][:], A["t2"][:], ALU.mult)
        yield
        P.ts("dve", A["yc"][:], A["yc"][:], pc("lnw"), pc("lnb"), op0=ALU.mult, op1=ALU.add)
        yield
        P.tt("pool", A["yc"][:], A["yc"][:], A["lw"][:], ALU.add)
        yield
        P.tt("pool", mixT[:, hp, :], A["yc"][:], d["g"][:], ALU.mult)
        yield

    for tc in range(NTC):
        for t4 in range(4):
            tt = tc * 4 + t4
            P.dma("sp", xt[t4 % 2][:], x[tt * 128:(tt + 1) * 128, :])
            norm_tile(P, xt[t4 % 2], gb, hx[t4 % 2], ss, rstd, junk=junk)
            ptv = B[0][:].bitcast(BF16).rearrange("p (a b) -> p a b", a=8)
            for dc in range(8):
                P.tr(ptv[:, dc, :], hx[t4 % 2][:, dc * 128:(dc + 1) * 128], c["idb"][:])
            P.cp("act", hT[:, :, t4 * 128:(t4 + 1) * 128], ptv)
        if tc > 0:
            P.cp("dve", praw[:, 0:20, 0:1], carry[:])
        for ct in range(22):
            wb = wbuf[ct % 3]
            P.dma("pool", wb[:], win.t[:, ct * 128:(ct + 1) * 128].rearrange("(c p) f -> p c f", p=128))
            pp = B[6 + ct % 2]
            for dc in range(8):
                P.mm(pp[:], wb[:, dc, :], hT[:, dc, :], start=(dc == 0), stop=(dc == 7))
            if ct < 20:
                P.cp("act" if ct % 2 else "dve", praw[:, ct, 1:513], pp[:])
            else:
                P.cp("act" if ct % 2 else "dve", qb[:, ct - 20, :], pp[:])
        P.cp("dve", carry[:], praw[:, 0:20, 512:513])
        def qf(t4):
            tsl = slice(t4 * 128, (t4 + 1) * 128)
            return lambda pr: qb[:, pr, tsl]
        def shift_gen():
            for k in range(20):
                tm = AS[k % G]["t1"]
                P.tt("pool", tm[:], praw[:, k, 0:512], praw[:, k, 1:513], ALU.subtract)
                P.stt(praw[:, k, 1:513], tm[:], mu_sb[:, k:k + 1], praw[:, k, 1:513])
                yield

        def xa_gen(i):
            for t4 in (i, i + 2):
                for _ in xas[i].gen(qf(t4), kT, v_sb, caT[:, :, t4 * 128:(t4 + 1) * 128]):
                    yield

        lockstep([xa_gen(0), xa_gen(1), shift_gen()])
        P.act(tw[0:64, :], praw[0:64, 18, 1:513], AF.Tanh)
        P.cp("act", tw[64:128, :], praw[64:128, 18, 1:513])
        P.act(sgd[:], praw[:, 19, 1:513], AF.Sigmoid)
        for grp in range(6 // G):
            hps = [grp * G + i for i in range(G)]
            if "prep" not in SKIP:
                lockstep([prep(hp, sets[i]) for i, hp in enumerate(hps)])
            for cc in range(8):
                if "core" not in SKIP:
                    lockstep([core(hp, cc, sets[i]) for i, hp in enumerate(hps)])
            if "post" not in SKIP:
                lockstep([post(hp, sets[i]) for i, hp in enumerate(hps)])
        kq = 0
        for nh in range(2):
            for kc in range(8):
                wo = wob[kq % 3]; kq += 1
                P.dma("pool", wo[:], wout.t[kc * 128:(kc + 1) * 128, nh * 512:(nh + 1) * 512])
                for t4 in range(4):
                    tsl = slice(t4 * 128, (t4 + 1) * 128)
                    lhs = mixT[:, kc, tsl] if kc < 6 else caT[:, kc - 6, tsl]
                    P.mm(B[4 + t4][:], lhs, wo[:], start=(kc == 0), stop=(kc == 7))
            for t4 in range(4):
                tt = tc * 4 + t4
                xo = xt[t4 % 2][:, (t4 // 2) * 512:(t4 // 2 + 1) * 512]
                P.dma("sp", xo, x[tt * 128:(tt + 1) * 128, nh * 512:(nh + 1) * 512])
                P.tt("dve", xo, B[4 + t4][:], xo, ALU.add)
                P.dma("sp", xout[tt * 128:(tt + 1) * 128, nh * 512:(nh + 1) * 512], xo)
    P.end_phase()


def build_all():
    P = Prog()
    x = P.inp("x", [S, D]); mem = P.inp("mem", [MEM, D]); out = P.out("out", [S, D])
    x1 = P.dram("x1", [S, D]); x2 = P.dram("x2", [S, D]); x3 = P.dram("x3", [S, D])
    P.begin_phase("a_"); emit_l0a(P, x, mem, x1)
    P.begin_phase("b_"); emit_ffn(P, x1, x2, 1, FD, False, False)
    P.begin_phase("c_"); emit_l1a(P, x2, mem, x3)
    P.begin_phase("d_"); emit_ffn(P, x3, out, NE, FE, True, True)
    P.finish()
    return P


def kernel(x, mem, norm_mix, norm_mem, w_kv_mem, w_out, norm_ffn, norm_final,
           w_in_a, s5_lam_re, s5_lam_im, s5_log_dt, s5_b_re, s5_b_im, s5_c_re, s5_c_im,
           s5_d, s5_w_glu, s5_b_glu, ffn_w_gate, ffn_w_up, ffn_w_down,
           w_in_b, rw_mu, rw_w0, rw_w2, rw_a0, rw_a2, rw_g2, rw_k_k, rw_k_a, rw_r_k,
           rw_lnx_w, rw_lnx_b, moe_w_router, moe_b_router, moe_w_gate, moe_w_up, moe_w_down):
    f = lambda a: np.ascontiguousarray(np.asarray(a, dtype=np.float32))
    fl = lambda a: f(a[0]).reshape(-1)
    x = f(x); mem = f(mem)
    base = dict(consts_np())
    pa = {"gmix": f(norm_mix[0]), "gmem": f(norm_mem[0]), "wkv": f(w_kv_mem[0]), "win": f(w_in_a[0]),
          "wout": f(w_out[0]), "lam_re": f(s5_lam_re[0]), "lam_im": f(s5_lam_im[0]), "log_dt": f(s5_log_dt[0]),
          "b_re": f(s5_b_re[0]), "b_im": f(s5_b_im[0]), "c_re": f(s5_c_re[0]), "c_im": f(s5_c_im[0]),
          "dsk": f(s5_d[0]).reshape(-1), "wglu": f(s5_w_glu[0]), "bglu": f(s5_b_glu[0]).reshape(-1)}
    pb = {"gnorm": f(norm_ffn[0]), "wg": f(ffn_w_gate), "wu": f(ffn_w_up), "wd": f(ffn_w_down)}
    pc = {"gmix": f(norm_mix[1]), "gmem": f(norm_mem[1]), "wkv": f(w_kv_mem[1]), "win": f(w_in_b[0]),
          "wout": f(w_out[1]), "mu": fl(rw_mu), "w0": fl(rw_w0), "w2": f(rw_w2[0]), "a0": fl(rw_a0),
          "a2": f(rw_a2[0]), "g2": f(rw_g2[0]), "k_k": fl(rw_k_k), "k_a": fl(rw_k_a), "r_k": fl(rw_r_k),
          "lnw": fl(rw_lnx_w), "lnb": fl(rw_lnx_b), **rwkv_consts_np()}
    pd = {"gnorm": f(norm_ffn[1]), "wg": f(moe_w_gate[0]), "wu": f(moe_w_up[0]), "wd": f(moe_w_down[0]),
          "wr": f(moe_w_router[0]), "br": f(moe_b_router[0]), "gfinal": f(norm_final)}
    for pre, d in (("a_", pa), ("b_", pb), ("c_", pc), ("d_", pd)):
        for k, v in d.items():
            base[pre + k] = v
    P = build_all()
    res = run(P, [{"x": x[b], "mem": mem[b], **base} for b in range(NCORES)])
    return np.stack([r["out"] for r in res], axis=0).astype(np.float32)
```

```python
import contextlib
import math
import numpy as np
import concourse.bass as bass
import concourse.mybir as mybir
from concourse.bass_utils import run_bass_kernel_spmd

F32 = mybir.dt.float32
BF16 = mybir.dt.bfloat16
I32 = mybir.dt.int32
AF = mybir.ActivationFunctionType
ALU = mybir.AluOpType
AX = mybir.AxisListType

NCORES = 8
D = 1024
S = 4096
MEM = 256
NT = S // 128
NTC = S // 512
C_MIX = 768
FD = 2816
FE = 3584
NE = 8


class Trk:
    __slots__ = ("w", "r")

    def __init__(self):
        self.w = {}
        self.r = {}


class T:
    def __init__(self, t, name):
        self.t = t
        self.name = name
        self.trk = Trk()
        self.dsem = {}
        self.is_psum = False

    def __getitem__(self, idx):
        return self.t[idx]


def _isap(x):
    return hasattr(x, "tensor") and hasattr(x, "ap")


class Prog:
    def __init__(self, same_engine_wait=True):
        self.nc = bass.Bass("TRN2", target_bir_lowering=False)
        self.es = contextlib.ExitStack()
        nc = self.nc
        self.eng = {"pe": nc.tensor, "act": nc.scalar, "dve": nc.vector, "pool": nc.gpsimd, "sp": nc.sync}
        self.esem, self.ecnt, self.waited = {}, {}, {}
        for e in self.eng:
            self.esem[e] = self.es.enter_context(nc.semaphore("es_" + e))
            self.ecnt[e] = 0
            self.waited[e] = {}
        self.sew = same_engine_wait
        self.ninst = 0
        self.byname = {}
        self.uid = 0
        self.prefix = ""
        self.sem_es = self.es
        self.dsem_pool = {}
        self.dsem_latest = {}
        self.phase_tiles = []
        self.cache = {}

    def _reg(self, t, name):
        w = T(t, name)
        self.byname[name] = w
        self.phase_tiles.append(w)
        return w

    def sb(self, name, shape, dt=F32, stack=None):
        name = self.prefix + name
        t = (stack or self.es).enter_context(self.nc.sbuf_tensor(name, list(shape), dt))
        return self._reg(t, name)

    def ps(self, name, shape, dt=F32, stack=None):
        name = self.prefix + name
        t = (stack or self.es).enter_context(self.nc.psum_tensor(name, list(shape), dt))
        w = self._reg(t, name)
        w.is_psum = True
        return w

    def dram(self, name, shape, dt=F32, kind="Internal", raw=False):
        if not raw:
            name = self.prefix + name
        t = self.nc.dram_tensor(name, list(shape), dt, kind=kind)
        return self._reg(t.ap(), name)

    def begin_phase(self, prefix):
        self.prefix = prefix
        self._saved_es = self.es
        self.es = contextlib.ExitStack()
        self.phase_tiles = []

    def end_phase(self):
        self.barrier()
        self.es.close()
        self.es = self._saved_es
        for tl in self.phase_tiles:
            for qt, (sem, cnt) in tl.dsem.items():
                self.dsem_pool.setdefault(qt, []).append((sem, cnt))
            tl.dsem = {}
        self.phase_tiles = []
        self.prefix = ""

    def inp(self, name, shape, dt=F32):
        return self.dram(name, shape, dt, kind="ExternalInput")

    def out(self, name, shape, dt=F32):
        return self.dram(name, shape, dt, kind="ExternalOutput")

    def _dsem(self, tile, qt):
        if qt not in tile.dsem:
            pool = self.dsem_pool.get(qt, [])
            if pool:
                sem, cnt = pool.pop()
            else:
                sem, cnt = self.sem_es.enter_context(self.nc.semaphore("ds_" + qt + "_" + tile.name)), 0
            tile.dsem[qt] = [sem, cnt]
        return tile.dsem[qt]

    def share_dsem(self, tiles):
        return tiles

    def _tl(self, aps):
        out = []
        for a in aps:
            if a is None:
                continue
            if isinstance(a, T):
                out.append(a)
            elif _isap(a):
                out.append(self.byname[a.tensor.name])
        return out

    def _deps(self, e, reads, writes, skip_waw=False):
        deps = {}

        def add(d):
            for sid, (sem, cnt) in d.items():
                if sid not in deps or deps[sid][1] < cnt:
                    deps[sid] = (sem, cnt)

        for tile in reads:
            add(tile.trk.w)
        for tile in writes:
            if not skip_waw:
                add(tile.trk.w)
            else:
                esids = {id(v) for v in self.esem.values()}
                add({k_: v_ for k_, v_ in tile.trk.w.items() if k_ in esids})
            add(tile.trk.r)
        engine = self.eng[e]
        wd = self.waited[e]
        own = id(self.esem[e])
        for sid, (sem, cnt) in deps.items():
            if sid == own and (e == "pe" or not self.sew):
                continue
            if wd.get(sid, 0) >= cnt:
                continue
            engine.wait_ge(sem, cnt)
            wd[sid] = cnt

    def _record(self, tok, reads, writes, accumulate=False):
        sem, cnt = tok
        for tile in reads:
            tile.trk.r[id(sem)] = (sem, cnt)
        for tile in writes:
            k = tile.trk
            if not accumulate:
                k.w = {}
            k.r = {}
            k.w[id(sem)] = (sem, cnt)

    def op(self, e, fn, ins=(), outs=(), accumulate=False):
        reads = self._tl(ins)
        writes = self._tl(outs)
        if e != "pe":
            writes = writes + [t_ for t_ in reads if t_.is_psum and t_ not in writes]
        self._deps(e, reads, writes)
        ins_ = fn(self.eng[e])
        self.ecnt[e] += 1
        ins_.then_inc(self.esem[e], 1)
        self.ninst += 1
        self._record((self.esem[e], self.ecnt[e]), reads, writes, accumulate)
        return ins_

    def dma(self, e, out_ap, in_ap, sem_tile=None, **kw):
        reads = self._tl([in_ap])
        writes = self._tl([out_ap])
        self._deps(e, reads, writes, skip_waw=True)
        if sem_tile is None:
            sem_tile = writes[0] if "SB" in type(out_ap.tensor).__name__ else reads[0]
        ent = self._dsem(sem_tile, "sw" if e == "pool" else "hw")
        sem = ent[0]
        i = self.eng[e].dma_start(out=out_ap, in_=in_ap, **kw)
        ent[1] += 16
        i.then_inc(sem, 16)
        self.dsem_latest[id(sem)] = (sem, ent[1])
        self.ninst += 1
        self._record((sem, ent[1]), reads, writes, accumulate=True)
        return i

    def barrier(self):
        for e in self.eng:
            engine = self.eng[e]
            wd = self.waited[e]
            for o in self.eng:
                if o == e:
                    continue
                c = self.ecnt[o]
                if c > wd.get(id(self.esem[o]), 0):
                    engine.wait_ge(self.esem[o], c)
                    wd[id(self.esem[o])] = c
            for sid, (sem, cnt) in self.dsem_latest.items():
                if cnt > wd.get(sid, 0):
                    engine.wait_ge(sem, cnt)
                    wd[sid] = cnt

    def finish(self):
        self.barrier()
        self.es.close()

    def mm(self, out, lhsT, rhs, start=True, stop=True):
        return self.op("pe", lambda en: en.matmul(out, lhsT, rhs, start=start, stop=stop),
                       ins=[lhsT, rhs], outs=[out], accumulate=not start)

    def tr(self, out, in_, ident):
        return self.op("pe", lambda en: en.transpose(out, in_, ident), ins=[in_, ident], outs=[out])

    def act(self, out, in_, func, bias=None, scale=1.0, accum=None):
        kw = {}
        if bias is not None:
            kw["bias"] = bias
        if accum is not None:
            kw["accum_out"] = accum
        return self.op("act", lambda en: en.activation(out=out, in_=in_, func=func, scale=scale, **kw),
                       ins=[in_, bias, scale], outs=[out, accum])

    def tt(self, e, out, in0, in1, op):
        return self.op(e, lambda en: en.tensor_tensor(out, in0, in1, op), ins=[in0, in1], outs=[out])

    def ts(self, e, out, in0, s1, s2=None, op0=ALU.mult, op1=None, accum=None):
        kw = {}
        if op1 is not None:
            kw["op1"] = op1
        if accum is not None:
            kw["accum_out"] = accum
        return self.op(e, lambda en: en.tensor_scalar(out, in0, s1, s2, op0, **kw),
                       ins=[in0, s1, s2], outs=[out, accum])

    def stt(self, out, in0, scalar, in1, op0=ALU.mult, op1=ALU.add):
        return self.op("dve", lambda en: en.scalar_tensor_tensor(out, in0, scalar, in1, op0, op1),
                       ins=[in0, scalar, in1], outs=[out])

    def cp(self, e, out, in_):
        if e == "act":
            return self.op("act", lambda en: en.copy(out, in_), ins=[in_], outs=[out])
        return self.op(e, lambda en: en.tensor_copy(out, in_), ins=[in_], outs=[out])

    def memset(self, e, ap, val):
        return self.op(e, lambda en: en.memset(ap, val), outs=[ap])

    def recip(self, out, in_):
        return self.op("dve", lambda en: en.reciprocal(out, in_), ins=[in_], outs=[out])

    def red(self, e, out, in_, op, axis=AX.X):
        return self.op(e, lambda en: en.tensor_reduce(out, in_, axis, op), ins=[in_], outs=[out])

    def scan(self, out, d0, d1, init, op0=ALU.mult, op1=ALU.add):
        return self.op("dve", lambda en: en.tensor_tensor_scan(out, d0, d1, init, op0, op1),
                       ins=[d0, d1, init], outs=[out])


def load_consts(P):
    c = {}
    if "idf_d" not in P.cache:
        P.cache["idf_d"] = P.dram("ident_f32", [128, 128], F32, kind="ExternalInput", raw=True)
        P.cache["idb_d"] = P.dram("ident_bf16", [128, 128], BF16, kind="ExternalInput", raw=True)
    c["idf_d"] = P.cache["idf_d"]; c["idb_d"] = P.cache["idb_d"]
    c["idf"] = P.sb("idf", [128, 128], F32)
    c["idb"] = P.sb("idb", [128, 128], BF16)
    P.dma("sp", c["idf"][:], c["idf_d"][:])
    P.dma("sp", c["idb"][:], c["idb_d"][:])
    return c


def norm_tile(P, xt, gb, hx, ss, rstd, eps=1e-6, junk=None):
    P.act(junk[:], xt[:], AF.Square, accum=ss[:])
    P.act(rstd[:], ss[:], AF.Sqrt, bias=None, scale=1.0 / D) if False else None
    P.ts("dve", rstd[:], ss[:], 1.0 / D, eps, op0=ALU.mult, op1=ALU.add)
    P.act(rstd[:], rstd[:], AF.Sqrt)
    P.recip(rstd[:], rstd[:])
    P.stt(hx[:], xt[:], rstd[:, 0:1], gb[:], op0=ALU.mult, op1=ALU.mult)


def build_ffn(n_exp, ff, moe, final_norm):
    P = Prog()
    xin = P.inp("xin", [S, D]); xout = P.out("xout", [S, D])
    P.begin_phase("")
    emit_ffn(P, xin, xout, n_exp, ff, moe, final_norm)
    P.finish()
    return P


def emit_ffn(P, xin, xout, n_exp, ff, moe, final_norm):
    nfc = ff // 128
    gn = P.inp("gnorm", [D])
    wg = P.inp("wg", [n_exp, D, ff])
    wu = P.inp("wu", [n_exp, D, ff])
    wd = P.inp("wd", [n_exp, ff, D])
    if moe:
        wr = P.inp("wr", [D, NE])
        br = P.inp("br", [NE])
    if final_norm:
        gf = P.inp("gfinal", [D])
    c = load_consts(P)

    ST = 1024
    NST = S // ST
    TPS = ST // 128
    gb = P.sb("gb", [128, D])
    P.dma("sp", gb[:], gn.t.partition_broadcast(128))
    if final_norm:
        gfb = P.sb("gfb", [128, D])
        P.dma("sp", gfb[:], gf.t.partition_broadcast(128))
    if moe:
        wr_sb = P.sb("wr_sb", [128, 8, NE])
        P.dma("sp", wr_sb[:], wr.t.rearrange("(c p) e -> p c e", p=128))
        brb = P.sb("brb", [128, NE])
        P.dma("sp", brb[:], br.t.partition_broadcast(128))
        gates_s = [P.sb(f"gates{i}", [128, TPS, NE]) for i in range(2)]
        hxf = P.sb("hxf", [128, D])
        hTf = P.sb("hTf", [128, 8, 128])
        lg = P.sb("lg", [128, NE]); eq = P.sb("eq", [128, NE]); l2 = P.sb("l2", [128, NE])
        m1 = P.sb("m1", [128, 1]); m2 = P.sb("m2", [128, 1]); nm1 = P.sb("nm1", [128, 1])
        ee = P.sb("ee", [128, NE]); den = P.sb("den", [128, 1])
        ps_r = P.ps("ps_r", [128, 8, 128])
        ps_l = P.ps("ps_l", [128, NE])
    xres_s = [[P.sb(f"xres{b}_{i}", [128, D]) for i in range(TPS)] for b in range(2)]
    hT_s = [P.sb(f"hT{b}", [128, 8, ST], BF16) for b in range(2)]
    hx = [P.sb(f"hx{i}", [128, D], BF16) for i in range(1)] * 2
    junk = P.sb("junk", [128, D], BF16)
    ss2 = P.sb("ss2", [128, 1]); rstd2 = P.sb("rstd2", [128, 1])
    ss = P.sb("ss", [128, 1]); rstd = P.sb("rstd", [128, 1])
    actT = P.sb("actT", [128, nfc, ST], BF16)
    wd_sb = [P.sb(f"wd_sb{i}", [128, 4, D], BF16) for i in range(2)]
    wgu = [P.sb(f"wgu{i}", [128, 2, 8, 128], BF16) for i in range(3)]
    sg = [P.sb(f"sg{i}", [128, 512], BF16) for i in range(2)]
    ps_t = [P.ps(f"ps_t{i}", [128, 8, 128], BF16) for i in range(1)]
    n_mm = 2 if moe else 3
    ps_g = [P.ps(f"ps_g{i}", [128, 512]) for i in range(n_mm)]
    ps_u = [P.ps(f"ps_u{i}", [128, 512]) for i in range(n_mm)]
    NWD = 4

    def norm_stage(st):
        xres = xres_s[st % 2]; hT = hT_s[st % 2]
        if moe:
            gates = gates_s[st % 2]
        for tt in range(TPS):
            r0 = st * ST + tt * 128
            xt = xres[tt]
            P.dma("sp", xt[:], xin[r0:r0 + 128, :])
            h = hx[tt % 2]
            norm_tile(P, xt, gb, h, ss, rstd, junk=junk)
            yield
            pt = ps_t[0]
            for dc in range(8):
                P.tr(pt[:, dc, :], h[:, dc * 128:(dc + 1) * 128], c["idb"][:])
            P.cp("act" if tt % 2 else "dve", hT[:, :, tt * 128:(tt + 1) * 128], pt[:])
            yield
            if moe:
                P.stt(hxf[:], xt[:], rstd[:, 0:1], gb[:], op0=ALU.mult, op1=ALU.mult)
                for dc in range(8):
                    P.tr(ps_r[:, dc, :], hxf[:, dc * 128:(dc + 1) * 128], c["idf"][:])
                P.cp("act", hTf[:], ps_r[:])
                yield
                for dc in range(8):
                    P.mm(ps_l[:], hTf[:, dc, :], wr_sb[:, dc, :], start=(dc == 0), stop=(dc == 7))
                P.tt("dve", lg[:], ps_l[:], brb[:], ALU.add)
                yield
                P.red("dve", m1[:], lg[:], ALU.max)
                P.ts("dve", eq[:], lg[:], m1[:, 0:1], None, op0=ALU.is_equal)
                P.stt(l2[:], eq[:], -1e30, lg[:], op0=ALU.mult, op1=ALU.add)
                P.red("dve", m2[:], l2[:], ALU.max)
                P.ts("dve", eq[:], lg[:], m2[:, 0:1], None, op0=ALU.is_ge)
                P.ts("dve", nm1[:], m1[:], -1.0, None, op0=ALU.mult)
                P.act(ee[:], lg[:], AF.Exp, bias=nm1[:, 0:1])
                P.tt("dve", ee[:], ee[:], eq[:], ALU.mult)
                P.red("dve", den[:], ee[:], ALU.add)
                P.recip(den[:], den[:])
                P.ts("dve", gates[:, tt, :], ee[:], den[:, 0:1], None, op0=ALU.mult)
                yield

    def store_stage(st):
        xres_ = xres_s[st % 2]
        for tt in range(TPS):
            r0 = st * ST + tt * 128
            if final_norm:
                hof = hxf if moe else (P.byname.get(P.prefix + "hof0") or P.sb("hof0", [128, D]))
                norm_tile(P, xres_[tt], gfb, hof, ss2, rstd2, junk=junk)
                P.dma("sp", xout[r0:r0 + 128, :], hof[:])
            else:
                P.dma("sp", xout[r0:r0 + 128, :], xres_[tt][:])
            yield

    sgen = iter(())
    for _ in norm_stage(0):
        pass
    for st in range(NST):
        xres = xres_s[st % 2]; hT = hT_s[st % 2]
        if moe:
            gates = gates_s[st % 2]
        ngen = norm_stage(st + 1) if st + 1 < NST else iter(())
        for ex in range(n_exp):
            k = 0
            for fc in range(nfc):
                w = wgu[fc % 3]
                next(sgen, None)
                next(ngen, None)
                P.dma("pool", w[:, 0], wg.t[ex, :, fc * 128:(fc + 1) * 128].rearrange("(c p) f -> p c f", p=128))
                P.dma("pool", w[:, 1], wu.t[ex, :, fc * 128:(fc + 1) * 128].rearrange("(c p) f -> p c f", p=128))
                for tq in range(ST // 512):
                    pg = ps_g[k % n_mm]; pu = ps_u[k % n_mm]; sgt = sg[k % 2]; k += 1
                    for dc in range(8):
                        P.mm(pg[:], w[:, 0, dc, :], hT[:, dc, tq * 512:(tq + 1) * 512], start=(dc == 0), stop=(dc == 7))
                    for dc in range(8):
                        P.mm(pu[:], w[:, 1, dc, :], hT[:, dc, tq * 512:(tq + 1) * 512], start=(dc == 0), stop=(dc == 7))
                    P.act(sgt[:], pg[:], AF.Silu)
                    P.tt("dve", actT[:, fc, tq * 512:(tq + 1) * 512], sgt[:], pu[:], ALU.mult)
            ngrp = (nfc + NWD - 1) // NWD
            for gq in range(ngrp):
                f0 = gq * NWD
                nf = min(NWD, nfc - f0)
                wds = wd_sb[gq % 2]
                P.dma("pool", wds[:, 0:nf, :], wd.t[ex, f0 * 128:(f0 + nf) * 128, :].rearrange("(c p) n -> p c n", p=128))
                for tt in range(TPS):
                    for nh in range(2):
                        pd = ps_g[k % n_mm] if (k % 2 == 0) else ps_u[k % n_mm]
                        k += 1
                        for j in range(nf):
                            P.mm(pd[:], actT[:, f0 + j, tt * 128:(tt + 1) * 128], wds[:, j, nh * 512:(nh + 1) * 512],
                                 start=(j == 0), stop=(j == nf - 1))
                        xs = xres[tt][:, nh * 512:(nh + 1) * 512]
                        if moe:
                            P.stt(xs, pd[:], gates[:, tt, ex:ex + 1], xs, op0=ALU.mult, op1=ALU.add)
                        else:
                            P.tt("dve", xs, pd[:], xs, ALU.add)
        for _ in ngen:
            pass
        for _ in sgen:
            pass
        sgen = store_stage(st)
    for _ in sgen:
        pass
    P.end_phase()


_CONST = None


def consts_np():
    global _CONST
    if _CONST is None:
        import ml_dtypes
        _CONST = {"ident_f32": np.eye(128, dtype=np.float32),
                  "ident_bf16": np.eye(128, dtype=np.float32).astype(ml_dtypes.bfloat16)}
    return _CONST


def run(P, in_maps):
    res = run_bass_kernel_spmd(P.nc, in_maps, core_ids=list(range(NCORES)))
    return res.results


def norm_transpose(P, c, src, nrows, gb, hT_of, tmp):
    xt2, hx2, junk, ss, rstd, pt = tmp
    for tt in range(nrows // 128):
        xt = xt2[tt % 2]
        h = hx2[tt % 2]
        P.dma("sp", xt[:], src[tt * 128:(tt + 1) * 128, :])
        norm_tile(P, xt, gb, h, ss, rstd, junk=junk)
        for dc in range(8):
            P.tr(pt[:, dc, :], h[:, dc * 128:(dc + 1) * 128], c["idb"][:])
        P.cp("act" if tt % 2 else "dve", hT_of(tt), pt[:])


def sin_reduced(P, out, ang, tmp):
    MAGIC = 12582912.0
    P.ts("dve", tmp, ang, 1.0 / (2 * math.pi), MAGIC, op0=ALU.mult, op1=ALU.add)
    P.ts("dve", tmp, tmp, -MAGIC, None, op0=ALU.add)
    P.stt(tmp, tmp, -2 * math.pi, ang, op0=ALU.mult, op1=ALU.add)
    P.ts("dve", tmp, tmp, math.pi, -math.pi, op0=ALU.min, op1=ALU.max)
    P.act(out, tmp, AF.Sin)


def kv_stage(P, c, mem, gmem, wkv, tmp, stack, kT, v_sb):
    gbm = P.sb("gbm", [128, D], stack=stack)
    P.dma("sp", gbm[:], gmem.t.partition_broadcast(128))
    wkv_sb = P.sb("wkv_sb", [128, 8, 512], BF16, stack=stack)
    P.dma("pool", wkv_sb[:], wkv.t.rearrange("(c p) n -> p c n", p=128))
    hTm = P.sb("hTm", [128, 8, MEM], BF16, stack=stack)
    norm_transpose(P, c, mem, MEM, gbm, lambda tt: hTm[:, :, tt * 128:(tt + 1) * 128], tmp)
    ps_kv = P.ps("ps_kv", [128, 256], stack=stack)
    for j in range(2):
        for dc in range(8):
            P.mm(ps_kv[:], wkv_sb[:, dc, j * 128:(j + 1) * 128], hTm[:, dc, :], start=(dc == 0), stop=(dc == 7))
        P.memset("dve", kT[j][:], 0.0)
        for hh in range(2):
            hs = slice(hh * 64, hh * 64 + 64)
            P.cp("dve", kT[j][hs, hh, :], ps_kv[hs, :])
    P.memset("dve", v_sb[:], 0.0)
    for mt in range(2):
        for dc in range(8):
            P.mm(ps_kv[:], hTm[:, dc, mt * 128:(mt + 1) * 128], wkv_sb[:, dc, 256:512], start=(dc == 0), stop=(dc == 7))
        for h in range(4):
            P.cp("dve", v_sb[:, mt, h, (h % 2) * 64:(h % 2) * 64 + 64], ps_kv[:, h * 64:(h + 1) * 64])


SKIP = set()


def lockstep(gens):
    gens = list(gens)
    while gens:
        for g in list(gens):
            try:
                next(g)
            except StopIteration:
                gens.remove(g)


class XAttn:
    def __init__(self, P, c, stack, banks=None, tag=""):
        self.P, self.c = P, c
        if banks is None:
            self.ps_s = [P.ps(f"ps_s{tag}{i}", [128, 2, 256], stack=stack) for i in range(2)]
            self.ps_pt = P.ps("ps_pt" + tag, [128, 8, 128], BF16, stack=stack)
            self.ps_o = P.ps("ps_o" + tag, [128, 2, 128], stack=stack)
            self.v_s = [t[:] for t in self.ps_s]
            self.v_pt = self.ps_pt[:]
            self.v_o = self.ps_o[:]
        else:
            b0, b1, b2, b3 = banks
            self.v_s = [b0[:].rearrange("p (a b) -> p a b", a=2), b1[:].rearrange("p (a b) -> p a b", a=2)]
            self.v_pt = b2[:].bitcast(BF16).rearrange("p (a b) -> p a b", a=8)
            self.v_o = b3[:, 0:256].rearrange("p (a b) -> p a b", a=2)
        sb = lambda n, sh, dt=F32: P.sb(n + tag, sh, dt, stack=stack)
        self.mx = sb("mx", [128, 4]); self.nmx = sb("nmx", [128, 4])
        self.sm = sb("sm", [128, 4]); self.rs = sb("rs", [128, 4])
        self.pexp = sb("pexp", [128, 4, 256], BF16)
        self.pn = sb("pn", [128, 4, 256], BF16)
        self.pT = sb("pT_sb", [128, 8, 128], BF16)

    def gen(self, q_of, kT, v_sb, out_ap):
        P, c = self.P, self.c
        mx, nmx, sm, rs, pexp, pn, pT = self.mx, self.nmx, self.sm, self.rs, self.pexp, self.pn, self.pT
        for h in range(4):
            P.mm(self.v_s[h // 2][:, h % 2, :], q_of(h // 2), kT[h // 2][:, h % 2, :])
        yield
        for pr in range(2):
            P.red("dve", mx[:, pr * 2:pr * 2 + 2], self.v_s[pr], ALU.max)
        P.ts("dve", nmx[:], mx[:], -0.125, None, op0=ALU.mult)
        yield
        for h in range(4):
            P.act(pexp[:, h, :], self.v_s[h // 2][:, h % 2, :], AF.Exp, bias=nmx[:, h:h + 1], scale=0.125,
                  accum=sm[:, h:h + 1])
        yield
        P.recip(rs[:], sm[:])
        for h in range(4):
            P.ts("pool" if h % 2 else "dve", pn[:, h, :], pexp[:, h, :], rs[:, h:h + 1], None, op0=ALU.mult)
        yield
        for h in range(4):
            for mt in range(2):
                P.tr(self.v_pt[:, h * 2 + mt, :], pn[:, h, mt * 128:(mt + 1) * 128], c["idb"][:])
        P.cp("act", pT[:], self.v_pt)
        yield
        for h in range(4):
            for mt in range(2):
                P.mm(self.v_o[:, h // 2, :], v_sb[:, mt, h, :], pT[:, h * 2 + mt, :],
                     start=(h % 2 == 0 and mt == 0), stop=(h % 2 == 1 and mt == 1))
        P.cp("dve", out_ap, self.v_o)
        yield

    def tile(self, q_of, kT, v_sb, out_ap):
        for _ in self.gen(q_of, kT, v_sb, out_ap):
            pass


def xattn_stage(P, c, qT, kT, v_sb, caT, stack):
    xas = [XAttn(P, c, stack, tag=f"_{i}") for i in range(2)]

    def tl(tt):
        tsl = slice(tt * 128, (tt + 1) * 128)
        return lambda pr: qT[pr][:, tsl]

    for t2 in range(0, NT, 2):
        lockstep([xas[i].gen(tl(t2 + i), kT, v_sb, caT[:, :, (t2 + i) * 128:(t2 + i + 1) * 128]) for i in range(2)])


def outproj_stage(P, x_src, xout, mcT_of, wout, stack):
    wout_sb = P.sb("wout_sb", [128, 8, D], BF16, stack=stack)
    P.dma("pool", wout_sb[:], wout.t.rearrange("(c p) n -> p c n", p=128))
    xo = [P.sb(f"xo{i}", [128, D], stack=stack) for i in range(2)]
    ps_x = [P.ps(f"ps_x{i}", [128, 512], stack=stack) for i in range(4)]
    for tt in range(NT):
        tsl = slice(tt * 128, (tt + 1) * 128)
        xt = xo[tt % 2]
        P.dma("sp", xt[:], x_src[tsl, :])
        for nh in range(2):
            pp = ps_x[(tt % 2) * 2 + nh]
            for kc in range(8):
                P.mm(pp[:], mcT_of(kc, tsl), wout_sb[:, kc, nh * 512:(nh + 1) * 512], start=(kc == 0), stop=(kc == 7))
            P.tt("dve", xt[:, nh * 512:(nh + 1) * 512], pp[:], xt[:, nh * 512:(nh + 1) * 512], ALU.add)
        P.dma("sp", xout[tsl, :], xt[:])


def build_l0a(stop_after=99):
    P = Prog()
    x = P.inp("x", [S, D]); mem = P.inp("mem", [MEM, D]); xout = P.out("xout", [S, D])
    P.begin_phase("")
    emit_l0a(P, x, mem, xout)
    P.finish()
    return P


def emit_l0a(P, x, mem, xout, stop_after=99):
    gmix = P.inp("gmix", [D]); gmem = P.inp("gmem", [D])
    wkv = P.inp("wkv", [D, 512]); win = P.inp("win", [D, D]); wout = P.inp("wout", [D, D])
    lam_re = P.inp("lam_re", [48, 64]); lam_im = P.inp("lam_im", [48, 64]); log_dt = P.inp("log_dt", [48])
    b_re = P.inp("b_re", [48, 64, 16]); b_im = P.inp("b_im", [48, 64, 16])
    c_re = P.inp("c_re", [48, 16, 64]); c_im = P.inp("c_im", [48, 16, 64])
    dsk = P.inp("dsk", [768]); wglu = P.inp("wglu", [48, 16, 16]); bglu = P.inp("bglu", [768])
    c = load_consts(P)
    uT = [P.sb(f"uT{i}", [128, S], BF16) for i in range(6)]
    caT = P.sb("caT", [128, 2, S], BF16)
    kT = [P.sb(f"kT{j}", [128, 2, MEM], BF16) for j in range(2)]
    v_sb = P.sb("v_sb", [128, 2, 4, 128], BF16)
    stq = contextlib.ExitStack()
    qT = [P.sb(f"qT{i}", [128, S], BF16, stack=stq) for i in range(2)]

    st1 = contextlib.ExitStack()
    xt2 = [P.sb(f"xt{i}", [128, D], stack=st1) for i in range(2)]
    hx2 = [P.sb(f"hxb{i}", [128, D], BF16, stack=st1) for i in range(2)]
    junk = P.sb("junk", [128, D], stack=st1)
    ss = P.sb("ss", [128, 1], stack=st1); rstd = P.sb("rstd", [128, 1], stack=st1)
    pt = P.ps("pt", [128, 8, 128], BF16, stack=st1)
    tmp = (xt2, hx2, junk, ss, rstd, pt)
    kv_stage(P, c, mem, gmem, wkv, tmp, st1, kT, v_sb)
    gb = P.sb("gb", [128, D], stack=st1)
    P.dma("sp", gb[:], gmix.t.partition_broadcast(128))
    win_sb = P.sb("win_sb", [128, 8, D], BF16, stack=st1)
    P.dma("pool", win_sb[:], win.t.rearrange("(c p) n -> p c n", p=128))
    hT = [P.sb(f"hT{i}", [128, 8, 512], BF16, stack=st1) for i in range(NTC)]
    norm_transpose(P, c, x, S, gb, lambda tt: hT[tt // 4][:, :, (tt % 4) * 128:(tt % 4 + 1) * 128], tmp)
    ps_p = [P.ps(f"ps_p{i}", [128, 512], stack=st1) for i in range(4)]
    k = 0
    for tc in range(NTC):
        for ct in range(8):
            pp = ps_p[k % 4]; k += 1
            for dc in range(8):
                P.mm(pp[:], win_sb[:, dc, ct * 128:(ct + 1) * 128], hT[tc][:, dc, :], start=(dc == 0), stop=(dc == 7))
            dst = uT[ct] if ct < 6 else qT[ct - 6]
            P.cp("act" if k % 2 else "dve", dst[:, tc * 512:(tc + 1) * 512], pp[:])
    P.barrier()
    st1.close()

    st2 = contextlib.ExitStack()
    xattn_stage(P, c, qT, kT, v_sb, caT, st2)
    P.barrier()
    st2.close()
    stq.close()

    st3 = contextlib.ExitStack()
    s5_stage(P, c, uT, lam_re, lam_im, log_dt, b_re, b_im, c_re, c_im, dsk, wglu, bglu, st3)
    P.barrier()
    st3.close()

    st4 = contextlib.ExitStack()
    outproj_stage(P, x, xout, lambda kc, tsl: (uT[kc][:, tsl] if kc < 6 else caT[:, kc - 6, tsl]), wout, st4)
    P.barrier()
    st4.close()
    P.end_phase()


def s5_stage(P, c, uT, lam_re, lam_im, log_dt, b_re, b_im, c_re, c_im, dsk, wglu, bglu, st):
    NJ = 24
    nc = P.nc

    pw_re = P.sb("s5_pwre", [128, NJ, 12], stack=st); pw_im = P.sb("s5_pwim", [128, NJ, 12], stack=st)
    pw_imn = P.sb("s5_pwimn", [128, NJ, 12], stack=st)
    WB = [P.sb("s5_WBre", [128, NJ, 128], BF16, stack=st), P.sb("s5_WBim", [128, NJ, 128], BF16, stack=st)]
    WC = [P.sb("s5_WCre", [128, NJ, 128], BF16, stack=st), P.sb("s5_WCim", [128, NJ, 128], BF16, stack=st)]
    glw = P.sb("s5_glw", [128, 6, 128], BF16, stack=st)
    dcol = P.sb("s5_d", [128, 6], stack=st); bgl = P.sb("s5_bgl", [128, 6], stack=st)
    stp = contextlib.ExitStack()
    cur = [stp]

    def sbt(name, shape, dt=F32):
        return P.sb(name, shape, dt, stack=cur[0])

    lr = sbt("s5_lr", [128, NJ]); li = sbt("s5_li", [128, NJ]); dt = sbt("s5_dt", [128, NJ])
    with nc.allow_non_contiguous_dma(reason="tiny param loads"):
        for gi in range(2):
            hs = slice(gi * 64, gi * 64 + 64)
            P.dma("sp", lr[hs, :], lam_re.t.rearrange("(j g) p -> g p j", g=2)[gi])
            P.dma("sp", li[hs, :], lam_im.t.rearrange("(j g) p -> g p j", g=2)[gi])
            P.dma("sp", dt[hs, :], log_dt.t.rearrange("(j g) -> g j", g=2)[gi].partition_broadcast(64))
    P.act(dt[:], dt[:], AF.Exp)
    t0 = sbt("s5_t0", [128, NJ]); t1 = sbt("s5_t1", [128, NJ]); t2 = sbt("s5_t2", [128, NJ])
    mag = sbt("s5_mag", [128, NJ]); ang = sbt("s5_ang", [128, NJ])
    sn = sbt("s5_sn", [128, NJ]); cs = sbt("s5_cs", [128, NJ])
    P.tt("dve", t0[:], lr[:], dt[:], ALU.mult)
    P.act(mag[:], t0[:], AF.Exp)
    P.tt("dve", ang[:], li[:], dt[:], ALU.mult)
    sin_reduced(P, sn[:], ang[:], t1[:])
    P.ts("dve", t2[:], ang[:], math.pi / 2, None, op0=ALU.add)
    sin_reduced(P, cs[:], t2[:], t1[:])
    P.tt("dve", pw_re[:, :, 0], mag[:], cs[:], ALU.mult)
    P.tt("dve", pw_im[:, :, 0], mag[:], sn[:], ALU.mult)
    for k in range(1, 12):
        ar = pw_re[:, :, k - 1]; ai = pw_im[:, :, k - 1]
        P.tt("dve", t0[:], ar, ar, ALU.mult)
        P.tt("dve", t1[:], ai, ai, ALU.mult)
        P.tt("dve", pw_re[:, :, k], t0[:], t1[:], ALU.subtract)
        P.tt("dve", t0[:], ar, ai, ALU.mult)
        P.ts("dve", pw_im[:, :, k], t0[:], 2.0, None, op0=ALU.mult)
    P.ts("dve", pw_imn[:], pw_im[:], -1.0, None, op0=ALU.mult)
    zre = sbt("s5_zre", [128, NJ, 1]); zim = sbt("s5_zim", [128, NJ, 1]); den = sbt("s5_den", [128, NJ])
    nre = sbt("s5_nre", [128, NJ])
    P.ts("dve", nre[:], pw_re[:, :, 0], -1.0, None, op0=ALU.add)
    nim = pw_im[:, :, 0]
    P.tt("dve", t0[:], lr[:], lr[:], ALU.mult)
    P.tt("dve", t1[:], li[:], li[:], ALU.mult)
    P.tt("dve", den[:], t0[:], t1[:], ALU.add)
    P.recip(den[:], den[:])
    P.tt("dve", t0[:], nre[:], lr[:], ALU.mult)
    P.tt("dve", t1[:], nim, li[:], ALU.mult)
    P.tt("dve", t0[:], t0[:], t1[:], ALU.add)
    P.tt("dve", zre[:, :, 0], t0[:], den[:], ALU.mult)
    P.tt("dve", t0[:], nim, lr[:], ALU.mult)
    P.tt("dve", t1[:], nre[:], li[:], ALU.mult)
    P.tt("dve", t0[:], t0[:], t1[:], ALU.subtract)
    P.tt("dve", zim[:, :, 0], t0[:], den[:], ALU.mult)
    braw_re = sbt("s5_brre", [128, NJ, 16]); braw_im = sbt("s5_brim", [128, NJ, 16])
    for gi in range(2):
        hs = slice(gi * 64, gi * 64 + 64)
        P.dma("sp", braw_re[hs, :, :], b_re.t.rearrange("(j g) p i -> g p j i", g=2)[gi])
        P.dma("sp", braw_im[hs, :, :], b_im.t.rearrange("(j g) p i -> g p j i", g=2)[gi])
    bb_re = sbt("s5_bbre", [128, NJ, 16]); bb_im = sbt("s5_bbim", [128, NJ, 16]); bt = sbt("s5_bt", [128, NJ, 16])
    zreb = zre[:].to_broadcast([128, NJ, 16]); zimb = zim[:].to_broadcast([128, NJ, 16])
    P.tt("dve", bb_re[:], braw_re[:], zreb, ALU.mult)
    P.tt("dve", bt[:], braw_im[:], zimb, ALU.mult)
    P.tt("dve", bb_re[:], bb_re[:], bt[:], ALU.subtract)
    P.tt("dve", bb_im[:], braw_im[:], zreb, ALU.mult)
    P.tt("dve", bt[:], braw_re[:], zimb, ALU.mult)
    P.tt("dve", bb_im[:], bb_im[:], bt[:], ALU.add)
    pad = sbt("s5_pad", [128, NJ, 128])
    ps_w = [P.ps(f"s5_psw{i}", [128, 4, 128], stack=stp) for i in range(2)]
    for ri, bb in enumerate((bb_re, bb_im)):
        P.memset("pool", pad[:], 0.0)
        padv = pad[:].rearrange("p (ct jq) c -> p ct jq c", jq=4)
        bbv = bb[:].rearrange("p (ct jq) i -> p ct jq i", jq=4)
        for gi in range(2):
            hs = slice(gi * 64, gi * 64 + 64)
            for jq in range(4):
                c0 = jq * 32 + gi * 16
                P.cp("pool", padv[hs, :, jq, c0:c0 + 16], bbv[hs, :, jq, :])
        for j4 in range(NJ // 4):
            pw = ps_w[j4 % 2]
            for q in range(4):
                P.tr(pw[:, q, :], pad[:, j4 * 4 + q, :], c["idf"][:])
            P.cp("act", WB[ri][:, j4 * 4:(j4 + 1) * 4, :], pw[:])
    for ri, cc in enumerate((c_re, c_im)):
        P.memset("pool", pad[:], 0.0)
        padv = pad[:].rearrange("p (ct jq) c -> p ct jq c", jq=4)
        for gi in range(2):
            for jq in range(4):
                p0 = jq * 32 + gi * 16
                src = cc.t.rearrange("(ct jq g) j p -> jq g j ct p", jq=4, g=2)[jq, gi]
                P.dma("sp", padv[p0:p0 + 16, :, jq, gi * 64:gi * 64 + 64], src)
        for j4 in range(NJ // 4):
            pw = ps_w[j4 % 2]
            for q in range(4):
                P.tr(pw[:, q, :], pad[:, j4 * 4 + q, :], c["idf"][:])
            if ri == 0:
                P.cp("act", WC[ri][:, j4 * 4:(j4 + 1) * 4, :], pw[:])
            else:
                P.act(WC[ri][:, j4 * 4:(j4 + 1) * 4, :], pw[:], AF.Copy, scale=-1.0)
    gl32 = sbt("s5_gl32", [128, 6, 128])
    P.memset("pool", gl32[:], 0.0)
    for g_l in range(8):
        src = wglu.t.rearrange("(ct gl) j k -> gl j ct k", gl=8)[g_l]
        P.dma("sp", gl32[g_l * 16:(g_l + 1) * 16, :, g_l * 16:(g_l + 1) * 16], src)
    P.cp("dve", glw[:], gl32[:])
    with nc.allow_non_contiguous_dma(reason="tiny param loads"):
        P.dma("sp", dcol[:], dsk.t.rearrange("(ct p) -> p ct", p=128))
        P.dma("sp", bgl[:], bglu.t.rearrange("(ct p) -> p ct", p=128))

    P.barrier()
    stp.close()
    cur[0] = st
    xr = [sbt(f"s5_xr{i}", [128, S]) for i in range(2)]
    xi = [sbt(f"s5_xi{i}", [128, S]) for i in range(2)]
    xr16 = sbt("s5_xr16", [128, S], BF16); xi16 = sbt("s5_xi16", [128, S], BF16)
    yacc = sbt("s5_yacc", [128, S])
    ps_b = [P.ps(f"s5_psb{i}", [128, 512], stack=st) for i in range(4)]
    ps_g = [P.ps(f"s5_psg{i}", [128, 512], stack=st) for i in range(2)]
    gy = [sbt(f"s5_gy{i}", [128, 512]) for i in range(1)] * 2
    g2 = [sbt(f"s5_g2{i}", [128, 512]) for i in range(1)] * 2
    gt = [sbt(f"s5_gt{i}", [128, 512]) for i in range(1)] * 2
    ge = [sbt(f"s5_ge{i}", [128, 512]) for i in range(1)] * 2
    ge16 = [sbt(f"s5_ge16{i}", [128, 512], BF16) for i in range(2)]
    sgl = [sbt(f"s5_sgl{i}", [128, 512]) for i in range(1)] * 2
    kkc = [0]

    def st_bu(jt):
        ct = jt // 4
        X = (xr[jt % 2], xi[jt % 2])
        for tc in range(NTC):
            tsl = slice(tc * 512, (tc + 1) * 512)
            for ri in range(2):
                pb = ps_b[kkc[0] % 4]; kkc[0] += 1
                P.mm(pb[:], WB[ri][:, jt, :], uT[ct][:, tsl])
                P.cp("act", X[ri][:, tsl], pb[:])

    def st_scan(jt):
        X = (xr[jt % 2], xi[jt % 2])
        for phase in range(2):
            ks = range(12) if phase == 0 else range(10, -1, -1)
            for k in ks:
                d = 1 << k
                vr = X[0][:].rearrange("p (n s) -> p n s", s=2 * d)
                vi = X[1][:].rearrange("p (n s) -> p n s", s=2 * d)
                if phase == 0:
                    dr, di = vr[:, :, 2 * d - 1], vi[:, :, 2 * d - 1]
                    sr, si = vr[:, :, d - 1], vi[:, :, d - 1]
                else:
                    dr, di = vr[:, 1:, d - 1], vi[:, 1:, d - 1]
                    sr, si = vr[:, :-1, 2 * d - 1], vi[:, :-1, 2 * d - 1]
                ar = pw_re[:, jt, k:k + 1]; ai = pw_im[:, jt, k:k + 1]; ain = pw_imn[:, jt, k:k + 1]
                P.stt(dr, sr, ar, dr)
                P.stt(dr, si, ain, dr)
                P.stt(di, si, ar, di)
                P.stt(di, sr, ai, di)

    def st_cast(jt):
        X = (xr[jt % 2], xi[jt % 2])
        P.cp("act", xr16[:], X[0][:])
        P.cp("pool", xi16[:], X[1][:])

    def st_cmm(jt):
        jq = jt % 4
        for tc in range(NTC):
            tsl = slice(tc * 512, (tc + 1) * 512)
            pb = ps_b[kkc[0] % 4]; kkc[0] += 1
            P.mm(pb[:], WC[0][:, jt, :], xr16[:, tsl], start=True, stop=False)
            P.mm(pb[:], WC[1][:, jt, :], xi16[:, tsl], start=False, stop=True)
            if jq == 0:
                P.cp("dve", yacc[:, tsl], pb[:])
            else:
                P.tt("dve", yacc[:, tsl], pb[:], yacc[:, tsl], ALU.add)

    def st_epi(ct):
        for tc in range(NTC):
            tsl = slice(tc * 512, (tc + 1) * 512)
            i2 = tc % 2
            P.stt(gy[i2][:], uT[ct][:, tsl], dcol[:, ct:ct + 1], yacc[:, tsl])
            P.tt("pool", g2[i2][:], gy[i2][:], gy[i2][:], ALU.mult)
            P.ts("pool", g2[i2][:], g2[i2][:], 0.044715, 1.0, op0=ALU.mult, op1=ALU.add)
            P.tt("pool", gt[i2][:], g2[i2][:], gy[i2][:], ALU.mult)
            P.act(gt[i2][:], gt[i2][:], AF.Sigmoid, scale=1.5957691216057308)
            P.tt("pool", ge[i2][:], gt[i2][:], gy[i2][:], ALU.mult)
            P.cp("pool", ge16[i2][:], ge[i2][:])
            P.mm(ps_g[i2][:], glw[:, ct, :], ge16[i2][:])
            P.act(sgl[i2][:], ps_g[i2][:], AF.Sigmoid, bias=bgl[:, ct:ct + 1])
            P.tt("pool", uT[ct][:, tsl], ge[i2][:], sgl[i2][:], ALU.mult)

    st_bu(0)
    for jt in range(NJ):
        if jt + 1 < NJ:
            st_bu(jt + 1)
        st_scan(jt)
        if jt >= 1:
            st_cmm(jt - 1)
            if (jt - 1) % 4 == 3:
                st_epi((jt - 1) // 4)
        st_cast(jt)
    st_cmm(NJ - 1)
    st_epi(5)


def rwkv_consts_np():
    s = np.arange(64)[:, None]; t = np.arange(64)[None, :]
    strict = (s < t).astype(np.float32); incl = (s <= t).astype(np.float32)
    maskA = np.concatenate([strict, incl, strict, incl, (t < s).astype(np.float32)], axis=1)
    maskZ = np.zeros((64, 2, 128), np.float32); maskZ[:, 0, 0:64] = 1; maskZ[:, 1, 64:128] = 1
    ones_bd = np.zeros((128, 128), np.float32); ones_bd[0:64, 0:64] = 1; ones_bd[64:, 64:] = 1
    seg = np.ones((128, 512), np.float32); seg[:, ::64] = 0
    return {"maskA": maskA, "maskZ": maskZ, "ones_bd": ones_bd, "segmask": seg}


def build_l1a():
    P = Prog()
    x = P.inp("x", [S, D]); mem = P.inp("mem", [MEM, D]); xout = P.out("xout", [S, D])
    P.begin_phase("")
    emit_l1a(P, x, mem, xout)
    P.finish()
    return P


def emit_l1a(P, x, mem, xout):
    nc = P.nc
    gmix = P.inp("gmix", [D]); gmem = P.inp("gmem", [D])
    wkv = P.inp("wkv", [D, 512]); win = P.inp("win", [D, 2816]); wout = P.inp("wout", [D, D])
    mu = P.inp("mu", [2560]); w0 = P.inp("w0", [768]); w2 = P.inp("w2", [64, 768]); a0 = P.inp("a0", [768])
    a2 = P.inp("a2", [64, 768]); g2 = P.inp("g2", [128, 768]); k_k = P.inp("k_k", [768]); k_a = P.inp("k_a", [768])
    r_k = P.inp("r_k", [768]); lnw = P.inp("lnw", [768]); lnb = P.inp("lnb", [768])
    maskA_d = P.inp("maskA", [64, 320]); maskZ_d = P.inp("maskZ", [64, 2, 128])
    ones_d = P.inp("ones_bd", [128, 128]); seg_d = P.inp("segmask", [128, 512])
    c = load_consts(P)
    kT = [P.sb(f"kT{j}", [128, 2, MEM], BF16) for j in range(2)]
    v_sb = P.sb("v_sb", [128, 2, 4, 128], BF16)
    st1 = contextlib.ExitStack()
    xt2 = [P.sb(f"xt{i}", [128, D], stack=st1) for i in range(2)]
    hx2 = [P.sb(f"hxb{i}", [128, D], BF16, stack=st1) for i in range(2)]
    junk = P.sb("junk", [128, D], stack=st1)
    ss = P.sb("ss", [128, 1], stack=st1); rstd = P.sb("rstd", [128, 1], stack=st1)
    pt = P.ps("pt", [128, 8, 128], BF16, stack=st1)
    kv_stage(P, c, mem, gmem, wkv, (xt2, hx2, junk, ss, rstd, pt), st1, kT, v_sb)
    P.barrier()
    st1.close()

    sb = P.sb
    B = [P.ps(f"bank{i}", [128, 512]) for i in range(8)]
    wbuf = [sb(f"wbuf{i}", [128, 8, 128], BF16) for i in range(3)]
    wob = [sb(f"wob{i}", [128, 512], BF16) for i in range(3)]
    gb = sb("gb", [128, D]); P.dma("sp", gb[:], gmix.t.partition_broadcast(128))
    lora = sb("lora", [128, 2, 768], BF16)
    P.memset("pool", lora[:], 0.0)
    P.dma("pool", lora[0:64, 0, :], w2[:]); P.dma("pool", lora[64:128, 1, :], a2[:])
    g2_sb = sb("g2_sb", [128, 768], BF16); P.dma("pool", g2_sb[:], g2[:])
    maskA = sb("maskA_sb", [64, 320]); P.dma("sp", maskA[:], maskA_d[:])
    maskZ = sb("maskZ_sb", [64, 2, 128]); P.dma("sp", maskZ[:], maskZ_d[:])
    ones_bd = sb("ones_sb", [128, 128]); P.dma("sp", ones_bd[:], ones_d[:])
    seg = sb("seg_sb", [128, 512]); P.dma("sp", seg[:], seg_d[:])
    prm = {}
    with nc.allow_non_contiguous_dma(reason="tiny param loads"):
        for nm, tns in (("w0", w0), ("a0", a0), ("k_k", k_k), ("k_a", k_a), ("r_k", r_k), ("lnw", lnw), ("lnb", lnb)):
            prm[nm] = sb("prm_" + nm, [128, 6])
            P.dma("sp", prm[nm][:], tns.t.rearrange("(c p) -> p c", p=128))
        mu_sb = sb("mu_sb", [128, 20])
        P.dma("sp", mu_sb[:], mu.t.rearrange("(c p) -> p c", p=128))
    Zbd = [sb(f"Zbd{i}", [128, 128]) for i in range(6)]
    Zb16 = [sb(f"Zb16_{i}", [128, 128], BF16) for i in range(6)]
    for z in Zbd + Zb16:
        P.memset("pool", z[:], 0.0)
    praw = sb("praw", [128, 20, 513])
    P.memset("pool", praw[:, :, 0:1], 0.0)
    carry = sb("carry", [128, 20, 1])
    hT = sb("hT", [128, 8, 512], BF16)
    xt = [sb(f"xl{i}", [128, D]) for i in range(2)]
    hx = [sb(f"hxl{i}", [128, D], BF16) for i in range(1)] * 2
    ss = sb("ssl", [128, 1]); rstd = sb("rstdl", [128, 1])
    qb = sb("qb", [128, 2, 512], BF16); caT = sb("caTc", [128, 2, 512], BF16)

    class _J:
        def __getitem__(self, idx):
            return qb[:].rearrange("p a b -> p (a b)")
    junk = _J()
    mixT = hT
    tw = sb("tw", [128, 512], BF16); sgd = sb("sgd", [128, 512], BF16)
    xas = [XAttn(P, c, P.es, banks=(B[1], B[2], B[0], B[3]), tag="_0"),
           XAttn(P, c, P.es, banks=(B[5], B[6], B[4], B[7]), tag="_1")]
    G = 3
    AS = []
    for i in range(G):
        A_ = {n: sb(f"rk{i}_" + n, [128, 512]) for n in
              ("lw", "a", "kk", "t1", "t2", "cl", "ecl", "encl", "eclp")}
        A_["yc"] = A_["kk"]
        A_["kkn"] = A_["kk"]
        AS.append(A_)
    tmp = AS[0]["t1"]
    sets = []
    for i in range(G):
        d = {}
        d["yT"] = sb(f"rk{i}_yT", [128, 512])
        for n in ("BT", "KT", "v16", "kmod", "g"):
            d[n] = sb(f"rk{i}_{n}", [128, 512], BF16)
        d["AR"] = sb(f"rk{i}_AR", [128, 8, 128], BF16)
        d["gam"] = sb(f"rk{i}_gam", [128, 8])
        zl = []
        for n in ("Vz", "Bz", "Kz", "Uz"):
            d[n] = sb(f"rk{i}_{n}", [128, 2, 128], BF16); zl.append(d[n])
        d["AM"] = [sb(f"rk{i}_AM{h}", [128, 320], BF16) for h in range(2)]
        d["Qp"] = [sb(f"rk{i}_Qp{j}", [128, 2, 64], BF16) for j in range(2)]
        d["Qn"] = [sb(f"rk{i}_Qn{j}", [128, 2, 64], BF16) for j in range(2)]
        d["Pp"] = sb(f"rk{i}_Pp", [128, 2, 64], BF16); d["T1"] = sb(f"rk{i}_T1", [128, 128], BF16)
        d["tmpZ"] = sb(f"rk{i}_tmpZ", [128, 64])
        zl += d["AM"] + d["Qp"] + d["Qn"] + [d["Pp"], d["T1"]]
        for n in ("BZc", "KZc", "AZc"):
            d[n] = sb(f"rk{i}_{n}", [128, 2, 64], BF16); zl.append(d[n])
        for z in zl:
            P.memset("pool", z[:], 0.0)
        d["bk"] = (B[i], B[i]) if G == 6 else (B[2 * i], B[2 * i + 1])
        d["A"] = AS[i]
        sets.append(d)
    EXPN = -math.exp(-0.5)

    def c3(ap):
        return ap.rearrange("p (c t) -> p c t", t=64)

    def prep(hp, d):
        A = d["A"]
        r_ = praw[:, hp, 1:513]; k_ = praw[:, 6 + hp, 1:513]
        cs = slice(hp * 128, (hp + 1) * 128)
        pc = lambda n: prm[n][:, hp:hp + 1]
        pb0, pb1 = d["bk"]
        P.mm(pb0[:], lora[:, 0, cs], tw[:])
        P.act(A["lw"][:], pb0[:], AF.Sigmoid, bias=pc("w0"))
        P.mm(pb1[:], lora[:, 1, cs], tw[:])
        P.act(A["a"][:], pb1[:], AF.Sigmoid, bias=pc("a0"))
        P.ts("dve", A["kk"][:], k_, pc("k_k"), None, op0=ALU.mult)
        yield
        P.ts("pool", A["lw"][:], A["lw"][:], EXPN, None, op0=ALU.mult)
        P.mm(pb0[:], g2_sb[:, cs], sgd[:])
        P.cp("act", d["g"][:], pb0[:])
        P.tt("pool", A["t1"][:], A["kk"][:], A["kk"][:], ALU.mult)
        yield
        P.scan(A["cl"][:], seg[:], A["lw"][:], 0.0)
        P.mm(pb1[:], ones_bd[:], A["t1"][:])
        P.ts("dve", A["t2"][:], pb1[:], 1e-24, None, op0=ALU.max)
        yield
        P.act(A["t2"][:], A["t2"][:], AF.Sqrt)
        P.ts("dve", A["t1"][:], A["a"][:], -1.0, pc("k_a"), op0=ALU.add, op1=ALU.mult)
        yield
        P.recip(A["t2"][:], A["t2"][:])
        P.act(A["ecl"][:], A["cl"][:], AF.Exp)
        P.act(A["encl"][:], A["cl"][:], AF.Exp, scale=-1.0)
        yield
        P.tt("pool", A["kkn"][:], A["kk"][:], A["t2"][:], ALU.mult)
        P.stt(d["kmod"][:], A["t1"][:], 1.0, k_, op0=ALU.add, op1=ALU.mult)
        yield
        P.tt("pool", A["t2"][:], A["cl"][:], A["lw"][:], ALU.subtract)
        P.tt("dve", d["AR"][:, :, 64:128], c3(r_), c3(A["ecl"][:]), ALU.mult)
        yield
        P.act(A["eclp"][:], A["t2"][:], AF.Exp)
        P.tt("pool", A["t1"][:], A["kkn"][:], A["a"][:], ALU.mult)
        P.tt("dve", d["KT"][:], d["kmod"][:], A["encl"][:], ALU.mult)
        yield
        P.stt(d["AR"][:, :, 0:64], c3(A["kkn"][:]), -1.0, c3(A["eclp"][:]), op0=ALU.mult, op1=ALU.mult)
        P.tt("pool", d["BT"][:], A["t1"][:], A["encl"][:], ALU.mult)
        P.cp("pool", d["gam"][:], c3(A["ecl"][:])[:, :, 63])
        P.cp("act", d["v16"][:], praw[:, 12 + hp, 1:513])
        yield

    def core(hp, cc, d):
        bkA, bkB = d["bk"]
        AR, BT, KT, AM, Pp, T1, tmpZ = d["AR"], d["BT"], d["KT"], d["AM"], d["Pp"], d["T1"], d["tmpZ"]
        Vz, Bz, Kz, Uz, Qp, Qn = d["Vz"], d["Bz"], d["Kz"], d["Uz"], d["Qp"], d["Qn"]
        csl = slice(cc * 64, (cc + 1) * 64)
        bT0 = bkA[0:64, 0:192].bitcast(BF16).rearrange("p (a b) -> p a b", a=3)
        bI = bkA[0:64, 0:256].rearrange("p (a b) -> p a b", a=4)
        bJ = bkB[0:64, 0:128].rearrange("p (a b) -> p a b", a=2)
        for h in range(2):
            hs = slice(h * 64, h * 64 + 64)
            P.cp("pool", d["BZc"][hs, h, :], BT[hs, csl])
            P.cp("act", d["KZc"][hs, h, :], KT[hs, csl])
            P.cp("pool", d["AZc"][hs, h, :], AR[hs, cc, 0:64])
        P.tr(bT0[:, 0, :], d["v16"][:, csl], c["idb"][:])
        P.tr(bT0[:, 1, :], BT[:, csl], c["idb"][:])
        P.tr(bT0[:, 2, :], KT[:, csl], c["idb"][:])
        for i, Xz in enumerate((Vz, Bz, Kz)):
            P.tt("dve", Xz[0:64], bT0[:, i, :].unsqueeze(1).to_broadcast([64, 2, 128]), maskZ[:], ALU.mult)
        yield
        for h in range(2):
            bA = bkB if h == 0 else bkA
            P.mm(bA[0:64, 0:128], d["BZc"][:, h, :], AR[:, cc, :])
            P.mm(bA[0:64, 128:256], d["KZc"][:, h, :], AR[:, cc, :])
            P.mm(bA[0:64, 256:320], d["AZc"][:, h, :], BT[:, csl])
            P.tt("dve", AM[h][0:64, :], bA[0:64, 0:320], maskA[:], ALU.mult)
        yield
        for h in range(2):
            P.tt("pool", Pp[0:64, h, :], AM[h][0:64, 0:64], c["idf"][0:64, 0:64], ALU.add)
        def q_step(lvl):
            o, n = (lvl - 1) % 2, lvl % 2
            for h in range(2):
                qn_o = AM[h][:, 256:320] if lvl == 1 else Qn[o][:, h, :]
                qp_o = AM[h][:, 0:64] if lvl == 1 else Qp[o][:, h, :]
                if lvl < 5:
                    P.mm(bI[:, h, :], qn_o, qp_o)
                P.mm(bI[:, 2 + h, :], qp_o, qn_o)
            if lvl < 5:
                P.cp("act", Qp[n][0:64], bI[:, 0:2, :])
            P.cp("act" if lvl % 2 else "dve", Qn[n][0:64], bI[:, 2:4, :])

        def p_step(lvl):
            n = lvl % 2
            for h in range(2):
                P.mm(bJ[:, h, :], Qn[n][:, h, :], Pp[:, h, :])
            P.tt("dve", Pp[0:64], bJ, Pp[0:64], ALU.add)

        q_step(1)
        yield
        for lvl in range(1, 6):
            p_step(lvl)
            if lvl < 5:
                q_step(lvl + 1)
            yield
        P.mm(bkA[0:64, 0:128], AR[:, cc, 0:64], Zb16[hp][:], start=True, stop=False)
        for h in range(2):
            P.mm(bkA[0:64, h * 64:(h + 1) * 64], AM[h][:, 128:192], Vz[:, h, h * 64:(h + 1) * 64],
                 start=False, stop=(h == 1))
        P.cp("act", T1[0:64], bkA[0:64, 0:128])
        yield
        for h in range(2):
            P.mm(bkB[0:64, h * 64:(h + 1) * 64], Pp[:, h, :], T1[:, h * 64:(h + 1) * 64])
        P.tt("dve", Uz[0:64], bkB[0:64, 0:128].unsqueeze(1).to_broadcast([64, 2, 128]), maskZ[:], ALU.mult)
        yield
        P.mm(bkA[:, 0:64], Zb16[hp][:], AR[:, cc, 64:128], start=True, stop=False)
        for h in range(2):
            P.mm(bkA[:, 0:64], Uz[:, h, :], AM[h][:, 64:128], start=False, stop=False)
        for h in range(2):
            P.mm(bkA[:, 0:64], Vz[:, h, :], AM[h][:, 192:256], start=False, stop=(h == 1))
        P.cp("act", d["yT"][:, csl], bkA[:, 0:64])
        yield
        for h in range(2):
            P.mm(bkB[:, 0:64], Bz[:, h, :], Uz[:, h, h * 64:(h + 1) * 64], start=(h == 0), stop=False)
        for h in range(2):
            P.mm(bkB[:, 0:64], Kz[:, h, :], Vz[:, h, h * 64:(h + 1) * 64], start=False, stop=(h == 1))
        P.act(tmpZ[:], bkB[:, 0:64], AF.Copy, scale=d["gam"][:, cc:cc + 1])
        for h in range(2):
            hs = slice(h * 64, h * 64 + 64)
            zs = Zbd[hp][hs, h * 64:(h + 1) * 64]
            P.stt(zs, zs, d["gam"][hs, cc:cc + 1], tmpZ[hs, :])
        P.cp("pool", Zb16[hp][:], Zbd[hp][:])
        yield

    def post(hp, d):
        A = d["A"]
        r_ = praw[:, hp, 1:513]; v_ = praw[:, 12 + hp, 1:513]
        pc = lambda n: prm[n][:, hp:hp + 1]
        pb0, pb1 = d["bk"]
        yT = d["yT"]
        P.mm(pb0[:], ones_bd[:], yT[:])
        P.stt(A["t1"][:], r_, pc("r_k"), d["kmod"][:], op0=ALU.mult, op1=ALU.mult)
        yield
        P.stt(A["yc"][:], pb0[:], -1.0 / 64, yT[:])
        P.mm(pb1[:], ones_bd[:], A["t1"][:])
        yield
        P.tt("pool", A["t1"][:], A["yc"][:], A["yc"][:], ALU.mult)
        P.tt("dve", A["lw"][:], pb1[:], v_, ALU.mult)
        yield
        P.mm(pb0[:], ones_bd[:], A["t1"][:])
        P.ts("dve", A["t2"][:], pb0[:], 1.0 / 64, 64e-5, op0=ALU.mult, op1=ALU.add)
        yield
        P.act(A["t2"][:], A["t2"][:], AF.Sqrt)
        yield
        P.recip(A["t2"][:], A["t2"][:])
        yield
        P.tt("pool", A["yc"][:], A["yc"][:], A["t2"][:], ALU.mult)
        yield
        P.ts("dve", A["yc"][:], A["yc"][:], pc("lnw"), pc("lnb"), op0=ALU.mult, op1=ALU.add)
        yield
        P.tt("pool", A["yc"][:], A["yc"][:], A["lw"][:], ALU.add)
        yield
        P.tt("pool", mixT[:, hp, :], A["yc"][:], d["g"][:], ALU.mult)
        yield

    for tc in range(NTC):
        for t4 in range(4):
            tt = tc * 4 + t4
            P.dma("sp", xt[t4 % 2][:], x[tt * 128:(tt + 1) * 128, :])
            norm_tile(P, xt[t4 % 2], gb, hx[t4 % 2], ss, rstd, junk=junk)
            ptv = B[0][:].bitcast(BF16).rearrange("p (a b) -> p a b", a=8)
            for dc in range(8):
                P.tr(ptv[:, dc, :], hx[t4 % 2][:, dc * 128:(dc + 1) * 128], c["idb"][:])
            P.cp("act", hT[:, :, t4 * 128:(t4 + 1) * 128], ptv)
        if tc > 0:
            P.cp("dve", praw[:, 0:20, 0:1], carry[:])
        for ct in range(22):
            wb = wbuf[ct % 3]
            P.dma("pool", wb[:], win.t[:, ct * 128:(ct + 1) * 128].rearrange("(c p) f -> p c f", p=128))
            pp = B[6 + ct % 2]
            for dc in range(8):
                P.mm(pp[:], wb[:, dc, :], hT[:, dc, :], start=(dc == 0), stop=(dc == 7))
            if ct < 20:
                P.cp("act" if ct % 2 else "dve", praw[:, ct, 1:513], pp[:])
            else:
                P.cp("act" if ct % 2 else "dve", qb[:, ct - 20, :], pp[:])
        P.cp("dve", carry[:], praw[:, 0:20, 512:513])
        def qf(t4):
            tsl = slice(t4 * 128, (t4 + 1) * 128)
            return lambda pr: qb[:, pr, tsl]
        def shift_gen():
            for k in range(20):
                tm = AS[k % G]["t1"]
                P.tt("pool", tm[:], praw[:, k, 0:512], praw[:, k, 1:513], ALU.subtract)
                P.stt(praw[:, k, 1:513], tm[:], mu_sb[:, k:k + 1], praw[:, k, 1:513])
                yield

        def xa_gen(i):
            for t4 in (i, i + 2):
                for _ in xas[i].gen(qf(t4), kT, v_sb, caT[:, :, t4 * 128:(t4 + 1) * 128]):
                    yield

        lockstep([xa_gen(0), xa_gen(1), shift_gen()])
        P.act(tw[0:64, :], praw[0:64, 18, 1:513], AF.Tanh)
        P.cp("act", tw[64:128, :], praw[64:128, 18, 1:513])
        P.act(sgd[:], praw[:, 19, 1:513], AF.Sigmoid)
        for grp in range(6 // G):
            hps = [grp * G + i for i in range(G)]
            if "prep" not in SKIP:
                lockstep([prep(hp, sets[i]) for i, hp in enumerate(hps)])
            for cc in range(8):
                if "core" not in SKIP:
                    lockstep([core(hp, cc, sets[i]) for i, hp in enumerate(hps)])
            if "post" not in SKIP:
                lockstep([post(hp, sets[i]) for i, hp in enumerate(hps)])
        kq = 0
        for nh in range(2):
            for kc in range(8):
                wo = wob[kq % 3]; kq += 1
                P.dma("pool", wo[:], wout.t[kc * 128:(kc + 1) * 128, nh * 512:(nh + 1) * 512])
                for t4 in range(4):
                    tsl = slice(t4 * 128, (t4 + 1) * 128)
                    lhs = mixT[:, kc, tsl] if kc < 6 else caT[:, kc - 6, tsl]
                    P.mm(B[4 + t4][:], lhs, wo[:], start=(kc == 0), stop=(kc == 7))
            for t4 in range(4):
                tt = tc * 4 + t4
                xo = xt[t4 % 2][:, (t4 // 2) * 512:(t4 // 2 + 1) * 512]
                P.dma("sp", xo, x[tt * 128:(tt + 1) * 128, nh * 512:(nh + 1) * 512])
                P.tt("dve", xo, B[4 + t4][:], xo, ALU.add)
                P.dma("sp", xout[tt * 128:(tt + 1) * 128, nh * 512:(nh + 1) * 512], xo)
    P.end_phase()


def build_all():
    P = Prog()
    x = P.inp("x", [S, D]); mem = P.inp("mem", [MEM, D]); out = P.out("out", [S, D])
    x1 = P.dram("x1", [S, D]); x2 = P.dram("x2", [S, D]); x3 = P.dram("x3", [S, D])
    P.begin_phase("a_"); emit_l0a(P, x, mem, x1)
    P.begin_phase("b_"); emit_ffn(P, x1, x2, 1, FD, False, False)
    P.begin_phase("c_"); emit_l1a(P, x2, mem, x3)
    P.begin_phase("d_"); emit_ffn(P, x3, out, NE, FE, True, True)
    P.finish()
    return P


def kernel(x, mem, norm_mix, norm_mem, w_kv_mem, w_out, norm_ffn, norm_final,
           w_in_a, s5_lam_re, s5_lam_im, s5_log_dt, s5_b_re, s5_b_im, s5_c_re, s5_c_im,
           s5_d, s5_w_glu, s5_b_glu, ffn_w_gate, ffn_w_up, ffn_w_down,
           w_in_b, rw_mu, rw_w0, rw_w2, rw_a0, rw_a2, rw_g2, rw_k_k, rw_k_a, rw_r_k,
           rw_lnx_w, rw_lnx_b, moe_w_router, moe_b_router, moe_w_gate, moe_w_up, moe_w_down):
    f = lambda a: np.ascontiguousarray(np.asarray(a, dtype=np.float32))
    fl = lambda a: f(a[0]).reshape(-1)
    x = f(x); mem = f(mem)
    base = dict(consts_np())
    pa = {"gmix": f(norm_mix[0]), "gmem": f(norm_mem[0]), "wkv": f(w_kv_mem[0]), "win": f(w_in_a[0]),
          "wout": f(w_out[0]), "lam_re": f(s5_lam_re[0]), "lam_im": f(s5_lam_im[0]), "log_dt": f(s5_log_dt[0]),
          "b_re": f(s5_b_re[0]), "b_im": f(s5_b_im[0]), "c_re": f(s5_c_re[0]), "c_im": f(s5_c_im[0]),
          "dsk": f(s5_d[0]).reshape(-1), "wglu": f(s5_w_glu[0]), "bglu": f(s5_b_glu[0]).reshape(-1)}
    pb = {"gnorm": f(norm_ffn[0]), "wg": f(ffn_w_gate), "wu": f(ffn_w_up), "wd": f(ffn_w_down)}
    pc = {"gmix": f(norm_mix[1]), "gmem": f(norm_mem[1]), "wkv": f(w_kv_mem[1]), "win": f(w_in_b[0]),
          "wout": f(w_out[1]), "mu": fl(rw_mu), "w0": fl(rw_w0), "w2": f(rw_w2[0]), "a0": fl(rw_a0),
          "a2": f(rw_a2[0]), "g2": f(rw_g2[0]), "k_k": fl(rw_k_k), "k_a": fl(rw_k_a), "r_k": fl(rw_r_k),
          "lnw": fl(rw_lnx_w), "lnb": fl(rw_lnx_b), **rwkv_consts_np()}
    pd = {"gnorm": f(norm_ffn[1]), "wg": f(moe_w_gate[0]), "wu": f(moe_w_up[0]), "wd": f(moe_w_down[0]),
          "wr": f(moe_w_router[0]), "br": f(moe_b_router[0]), "gfinal": f(norm_final)}
    for pre, d in (("a_", pa), ("b_", pb), ("c_", pc), ("d_", pd)):
        for k, v in d.items():
            base[pre + k] = v
    P = build_all()
    res = run(P, [{"x": x[b], "mem": mem[b], **base} for b in range(NCORES)])
    return np.stack([r["out"] for r in res], axis=0).astype(np.float32)
```
